# Optimizing a Trainium2 kernel written in Bass

```python
import jax, jax.numpy as jnp
from jax import lax
import numpy as np

D_MODEL = 2048
BATCH = 8
SEQ = 2048
DEPTH = 2

HEAD_DIM = 128
N_MIXERS = 4
GROUP_WIDTH = D_MODEL // N_MIXERS
GROUP_HEADS = GROUP_WIDTH // HEAD_DIM
MIX_WIDTH = N_MIXERS * GROUP_WIDTH

MLA_Q_RANK = 512
MLA_KV_RANK = 256
MLA_NOPE_DIM = 128
MLA_ROPE_DIM = 64
MLA_V_DIM = HEAD_DIM
MLA_QK_DIM = MLA_NOPE_DIM + MLA_ROPE_DIM

RET_CHUNK = 128
SB_Q_BLOCK = 128
ATTN_Q_BLOCK = 128
MOBA_BLOCK = 256
MOBA_TOPK = 3
MOBA_Q_CHUNK = 16

ROPE_THETA = 10000.0
NORM_EPS = 1e-6
NEG = -1e30

FFN_DIM = 5632
N_EXPERTS = 8
MOE_TOPK = 2
MOE_GROUP = 512
N_DENSE = (DEPTH + 1) // 2
N_MOE = DEPTH // 2

IN_SPLITS = (MLA_Q_RANK, MLA_KV_RANK, MLA_ROPE_DIM) + (GROUP_WIDTH,) * 10
IN_WIDTH = MLA_Q_RANK + MLA_KV_RANK + MLA_ROPE_DIM + 10 * GROUP_WIDTH

kernel_name = "hymba_style_mla_retnet_stickbreak_moba_moe"


def rms_norm(x, g):
    xf = x.astype(jnp.float32)
    y = xf * lax.rsqrt(jnp.mean(xf * xf, axis=-1, keepdims=True) + NORM_EPS)
    return (y * g.astype(jnp.float32)).astype(x.dtype)


def modulate(h, shift, scale):
    return h * (1.0 + scale[:, None, :]) + shift[:, None, :]


def rope_tables(positions, dim):
    inv_freq = ROPE_THETA ** (-jnp.arange(0, dim, 2, dtype=jnp.float32) / dim)
    ang = positions.astype(jnp.float32)[:, None] * inv_freq[None, :]
    return jnp.cos(ang), jnp.sin(ang)


def apply_rope(x, cos, sin):
    half = x.shape[-1] // 2
    x1 = x[..., :half].astype(jnp.float32)
    x2 = x[..., half:].astype(jnp.float32)
    out = jnp.concatenate([x1 * cos - x2 * sin, x2 * cos + x1 * sin], axis=-1)
    return out.astype(x.dtype)


def split_heads(t, n_heads):
    b, s, _ = t.shape
    return t.reshape(b, s, n_heads, -1).transpose(0, 2, 1, 3)


def merge_heads(t):
    b, h, s, d = t.shape
    return t.transpose(0, 2, 1, 3).reshape(b, s, h * d)


def causal_softmax_attention(q, k, v, scale):
    b, h, s, dk = q.shape
    dv = v.shape[-1]
    nq = s // ATTN_Q_BLOCK
    qb = q.reshape(b, h, nq, ATTN_Q_BLOCK, dk).transpose(2, 0, 1, 3, 4)
    kpos = jnp.arange(s)

    def step(args):
        q_i, i = args
        sc = jnp.einsum('bhqd,bhkd->bhqk', q_i, k, preferred_element_type=jnp.float32) * scale
        qpos = i * ATTN_Q_BLOCK + jnp.arange(ATTN_Q_BLOCK)
        sc = jnp.where(kpos[None, :] <= qpos[:, None], sc, NEG)
        p = jax.nn.softmax(sc, axis=-1)
        return jnp.einsum('bhqk,bhkd->bhqd', p.astype(v.dtype), v)

    out = lax.map(step, (qb, jnp.arange(nq)))
    return out.transpose(1, 2, 0, 3, 4).reshape(b, h, s, dv)


def mla_mixer(c_q, c_kv, k_pe, q_norm_g, kv_norm_g, w_uq, w_ukv, q_head_g, k_head_g, cos_pe, sin_pe):
    b, s, _ = c_q.shape
    q = split_heads(rms_norm(c_q, q_norm_g) @ w_uq, GROUP_HEADS)
    kv = split_heads(rms_norm(c_kv, kv_norm_g) @ w_ukv, GROUP_HEADS)
    k_nope, v = kv[..., :MLA_NOPE_DIM], kv[..., MLA_NOPE_DIM:]
    k_rope = jnp.broadcast_to(k_pe[:, None], (b, GROUP_HEADS, s, MLA_ROPE_DIM))
    k = jnp.concatenate([k_nope, k_rope], axis=-1)
    q = rms_norm(q, q_head_g)
    k = rms_norm(k, k_head_g)
    q = jnp.concatenate([q[..., :MLA_NOPE_DIM], apply_rope(q[..., MLA_NOPE_DIM:], cos_pe, sin_pe)], axis=-1)
    k = jnp.concatenate([k[..., :MLA_NOPE_DIM], apply_rope(k[..., MLA_NOPE_DIM:], cos_pe, sin_pe)], axis=-1)
    return merge_heads(causal_softmax_attention(q, k, v, MLA_QK_DIM ** -0.5))


def retention_mixer(q, k, v, g, cos, sin):
    f32 = jnp.float32
    q = apply_rope(split_heads(q, GROUP_HEADS), cos, sin).astype(f32)
    k = apply_rope(split_heads(k, GROUP_HEADS), cos, sin).astype(f32) * (HEAD_DIM ** -0.5)
    v = split_heads(v, GROUP_HEADS).astype(f32)
    b, h, s, d = q.shape
    log_gamma = jnp.log(1.0 - 2.0 ** (-5.0 - jnp.arange(h, dtype=f32)))
    n = s // RET_CHUNK
    idx = jnp.arange(RET_CHUNK, dtype=f32)
    rel = idx[:, None] - idx[None, :]
    intra_decay = jnp.where(rel >= 0, jnp.exp(jnp.maximum(rel, 0.0)[None] * log_gamma[:, None, None]), 0.0)
    query_decay = jnp.exp((idx + 1.0)[None, :] * log_gamma[:, None])
    key_decay = jnp.exp((RET_CHUNK - 1.0 - idx)[None, :] * log_gamma[:, None])
    chunk_decay = jnp.exp(RET_CHUNK * log_gamma)
    qc = q.reshape(b, h, n, RET_CHUNK, d)
    kc = k.reshape(b, h, n, RET_CHUNK, d)
    vc = v.reshape(b, h, n, RET_CHUNK, d)
    scores = jnp.einsum('bhnid,bhnjd->bhnij', qc, kc) * intra_decay[None, :, None]
    intra = jnp.einsum('bhnij,bhnjd->bhnid', scores, vc)

    def step(state, xs):
        q_n, k_n, v_n = xs
        inter_n = jnp.einsum('bhid,bhde->bhie', q_n, state) * query_decay[None, :, :, None]
        state = state * chunk_decay[None, :, None, None] + jnp.einsum(
            'bhjd,bhje->bhde', k_n * key_decay[None, :, :, None], v_n)
        return state, inter_n

    init = jnp.zeros((b, h, d, d), f32)
    _, inter = lax.scan(step, init, (qc.transpose(2, 0, 1, 3, 4), kc.transpose(2, 0, 1, 3, 4),
                                     vc.transpose(2, 0, 1, 3, 4)))
    y = (intra + inter.transpose(1, 2, 0, 3, 4)).reshape(b, h, s, d)
    mu = jnp.mean(y, axis=-1, keepdims=True)
    var = jnp.mean(jnp.square(y - mu), axis=-1, keepdims=True)
    y = merge_heads((y - mu) * lax.rsqrt(var + NORM_EPS))
    return (jax.nn.silu(g.astype(f32)) * y).astype(g.dtype)


def stick_breaking_mixer(q, k, v):
    q = split_heads(q, GROUP_HEADS)
    k = split_heads(k, GROUP_HEADS)
    v = split_heads(v, GROUP_HEADS)
    b, h, s, d = q.shape
    nq = s // SB_Q_BLOCK
    qb = q.reshape(b, h, nq, SB_Q_BLOCK, d).transpose(2, 0, 1, 3, 4)
    kpos = jnp.arange(s)
    scale = d ** -0.5

    def step(args):
        q_i, i = args
        z = jnp.einsum('bhqd,bhkd->bhqk', q_i, k, preferred_element_type=jnp.float32) * scale
        qpos = i * SB_Q_BLOCK + jnp.arange(SB_Q_BLOCK)
        strict = kpos[None, :] < qpos[:, None]
        log_1m = jnp.where(strict, jax.nn.log_sigmoid(-z), 0.0)
        suffix = lax.cumsum(log_1m, axis=3, reverse=True) - log_1m
        a = jnp.where(strict, jnp.exp(jax.nn.log_sigmoid(z) + suffix), 0.0)
        return jnp.einsum('bhqk,bhkd->bhqd', a.astype(v.dtype), v)

    out = lax.map(step, (qb, jnp.arange(nq)))
    return merge_heads(out.transpose(1, 2, 0, 3, 4).reshape(b, h, s, d))


def moba_mixer(q, k, v, q_head_g, k_head_g, cos, sin):
    q = apply_rope(rms_norm(split_heads(q, GROUP_HEADS), q_head_g), cos, sin)
    k = apply_rope(rms_norm(split_heads(k, GROUP_HEADS), k_head_g), cos, sin)
    v = split_heads(v, GROUP_HEADS)
    b, h, s, d = q.shape
    nb = -(-s // MOBA_BLOCK)
    pad = nb * MOBA_BLOCK - s
    kb = jnp.pad(k, ((0, 0), (0, 0), (0, pad), (0, 0))).reshape(b, h, nb, MOBA_BLOCK, d)
    vb = jnp.pad(v, ((0, 0), (0, 0), (0, pad), (0, 0))).reshape(b, h, nb, MOBA_BLOCK, d)
    k_mean = jnp.mean(kb.astype(jnp.float32), axis=3)
    topk = min(MOBA_TOPK, nb)
    nc = s // MOBA_Q_CHUNK
    qc = q.reshape(b, h, nc, MOBA_Q_CHUNK, d).transpose(2, 0, 1, 3, 4)
    b_idx = jnp.arange(b)[:, None, None, None]
    h_idx = jnp.arange(h)[None, :, None, None]
    blk_ids = jnp.arange(nb)
    in_blk = jnp.arange(MOBA_BLOCK)
    scale = d ** -0.5
    n_sel = topk * MOBA_BLOCK

    def step(args):
        q_i, i = args
        q0 = i * MOBA_Q_CHUNK
        cur = q0 // MOBA_BLOCK
        qpos = q0 + jnp.arange(MOBA_Q_CHUNK)
        gate = jnp.einsum('bhqd,bhnd->bhqn', q_i.astype(jnp.float32), k_mean)
        gate = jnp.where(blk_ids < cur, gate, -jnp.inf)
        _, sel = lax.top_k(gate, topk)
        sel_ok = sel < cur
        k_sel = kb[b_idx, h_idx, sel]
        v_sel = vb[b_idx, h_idx, sel]
        s_sel = jnp.einsum('bhqd,bhqnkd->bhqnk', q_i, k_sel, preferred_element_type=jnp.float32) * scale
        s_sel = jnp.where(sel_ok[..., None], s_sel, NEG).reshape(b, h, MOBA_Q_CHUNK, n_sel)
        k_own = lax.dynamic_index_in_dim(kb, cur, axis=2, keepdims=False)
        v_own = lax.dynamic_index_in_dim(vb, cur, axis=2, keepdims=False)
        s_own = jnp.einsum('bhqd,bhkd->bhqk', q_i, k_own, preferred_element_type=jnp.float32) * scale
        own_pos = cur * MOBA_BLOCK + in_blk
        s_own = jnp.where(own_pos[None, :] <= qpos[:, None], s_own, NEG)
        p = jax.nn.softmax(jnp.concatenate([s_sel, s_own], axis=-1), axis=-1)
        p_sel = p[..., :n_sel].reshape(b, h, MOBA_Q_CHUNK, topk, MOBA_BLOCK).astype(v.dtype)
        p_own = p[..., n_sel:].astype(v.dtype)
        return (jnp.einsum('bhqnk,bhqnkd->bhqd', p_sel, v_sel)
                + jnp.einsum('bhqk,bhkd->bhqd', p_own, v_own))

    out = lax.map(step, (qc, jnp.arange(nc)))
    return merge_heads(out.transpose(1, 2, 0, 3, 4).reshape(b, h, s, d))


def hybrid_mixer(h, w_in, mla_q_norm_g, mla_kv_norm_g, mla_w_uq, mla_w_ukv, mla_q_head_g, mla_k_head_g,
                 moba_q_head_g, moba_k_head_g, group_norm_g, w_out, cos_pe, sin_pe, cos_full, sin_full):
    proj = h @ w_in
    points = []
    acc = 0
    for w in IN_SPLITS[:-1]:
        acc += w
        points.append(acc)
    (c_q, c_kv, k_pe, r_q, r_k, r_v, r_g, sb_q, sb_k, sb_v,
     mb_q, mb_k, mb_v) = jnp.split(proj, points, axis=-1)
    y_mla = mla_mixer(c_q, c_kv, k_pe, mla_q_norm_g, mla_kv_norm_g, mla_w_uq, mla_w_ukv,
                      mla_q_head_g, mla_k_head_g, cos_pe, sin_pe)
    y_ret = retention_mixer(r_q, r_k, r_v, r_g, cos_full, sin_full)
    y_sb = stick_breaking_mixer(sb_q, sb_k, sb_v)
    y_moba = moba_mixer(mb_q, mb_k, mb_v, moba_q_head_g, moba_k_head_g, cos_full, sin_full)
    groups = [rms_norm(y_mla, group_norm_g[0]), rms_norm(y_ret, group_norm_g[1]),
              rms_norm(y_sb, group_norm_g[2]), rms_norm(y_moba, group_norm_g[3])]
    return jnp.concatenate(groups, axis=-1) @ w_out


def swiglu(h, w1, w3, w2):
    return (jax.nn.silu(h @ w1) * (h @ w3)) @ w2


def moe_swiglu(h, router_w, w1, w3, w2):
    b, s, d = h.shape
    t = b * s
    xf = h.reshape(t, d)
    logits = jnp.einsum('td,de->te', xf, router_w, preferred_element_type=jnp.float32)
    top_logits, top_idx = lax.top_k(logits, MOE_TOPK)
    gates = jax.nn.softmax(top_logits, axis=-1)
    n_slots = t * MOE_TOPK
    flat_e = top_idx.reshape(-1)
    flat_t = jnp.arange(n_slots) // MOE_TOPK
    flat_g = gates.reshape(-1)
    order = jnp.argsort(flat_e)
    sorted_e = flat_e[order]
    counts = jnp.bincount(flat_e, length=N_EXPERTS)
    starts = jnp.cumsum(counts) - counts
    padded = (counts + MOE_GROUP - 1) // MOE_GROUP * MOE_GROUP
    pad_ends = jnp.cumsum(padded)
    pad_starts = pad_ends - padded
    dest = pad_starts[sorted_e] + jnp.arange(n_slots) - starts[sorted_e]
    n_chunks = -(-n_slots // MOE_GROUP) + N_EXPERTS
    n_buf = n_chunks * MOE_GROUP
    buf_t = jnp.full((n_buf,), t, jnp.int32).at[dest].set(flat_t[order].astype(jnp.int32))
    buf_g = jnp.zeros((n_buf,), jnp.float32).at[dest].set(flat_g[order])
    chunk_e = jnp.minimum(jnp.searchsorted(pad_ends, jnp.arange(n_chunks) * MOE_GROUP, side='right'),
                          N_EXPERTS - 1)
    x_pad = jnp.concatenate([xf, jnp.zeros((1, d), xf.dtype)], axis=0)
    x_buf = x_pad[buf_t].reshape(n_chunks, MOE_GROUP, d)

    def expert_chunk(args):
        xc, e = args
        return swiglu(xc, w1[e], w3[e], w2[e])

    y_buf = lax.map(expert_chunk, (x_buf, chunk_e)).reshape(n_buf, d)
    out = jnp.zeros((t + 1, d), jnp.float32).at[buf_t].add(y_buf.astype(jnp.float32) * buf_g[:, None])
    return out[:t].reshape(b, s, d).astype(h.dtype)


def setup_inputs(seed: int = 0) -> dict:
    key = jax.random.key(seed)
    ks = jax.random.split(key, 26)
    f32 = jnp.float32

    def nrm(k, shape, std):
        return jax.random.normal(k, shape, f32) * std

    def gain(k, shape):
        return 1.0 + 0.02 * jax.random.normal(k, shape, f32)

    return {
        "x": nrm(ks[0], (BATCH, SEQ, D_MODEL), 1.0),
        "c": nrm(ks[1], (BATCH, D_MODEL), 1.0),
        "positions": jnp.arange(SEQ, dtype=jnp.int32),
        "ada_w": nrm(ks[2], (DEPTH, D_MODEL, 6 * D_MODEL), 0.5 * D_MODEL ** -0.5),
        "ada_b": nrm(ks[3], (DEPTH, 6 * D_MODEL), 0.02),
        "norm_mix_g": gain(ks[4], (DEPTH, D_MODEL)),
        "norm_ffn_g": gain(ks[5], (DEPTH, D_MODEL)),
        "w_in": nrm(ks[6], (DEPTH, D_MODEL, IN_WIDTH), D_MODEL ** -0.5),
        "mla_q_norm_g": gain(ks[7], (DEPTH, MLA_Q_RANK)),
        "mla_kv_norm_g": gain(ks[8], (DEPTH, MLA_KV_RANK)),
        "mla_w_uq": nrm(ks[9], (DEPTH, MLA_Q_RANK, GROUP_HEADS * MLA_QK_DIM), MLA_Q_RANK ** -0.5),
        "mla_w_ukv": nrm(ks[10], (DEPTH, MLA_KV_RANK, GROUP_HEADS * (MLA_NOPE_DIM + MLA_V_DIM)), MLA_KV_RANK ** -0.5),
        "mla_q_head_g": gain(ks[11], (DEPTH, MLA_QK_DIM)),
        "mla_k_head_g": gain(ks[12], (DEPTH, MLA_QK_DIM)),
        "moba_q_head_g": gain(ks[13], (DEPTH, HEAD_DIM)),
        "moba_k_head_g": gain(ks[14], (DEPTH, HEAD_DIM)),
        "group_norm_g": gain(ks[15], (DEPTH, N_MIXERS, GROUP_WIDTH)),
        "w_out": nrm(ks[16], (DEPTH, MIX_WIDTH, D_MODEL), MIX_WIDTH ** -0.5),
        "ffn_w1": nrm(ks[17], (N_DENSE, D_MODEL, FFN_DIM), D_MODEL ** -0.5),
        "ffn_w3": nrm(ks[18], (N_DENSE, D_MODEL, FFN_DIM), D_MODEL ** -0.5),
        "ffn_w2": nrm(ks[19], (N_DENSE, FFN_DIM, D_MODEL), FFN_DIM ** -0.5),
        "router_w": nrm(ks[20], (N_MOE, D_MODEL, N_EXPERTS), D_MODEL ** -0.5),
        "moe_w1": nrm(ks[21], (N_MOE, N_EXPERTS, D_MODEL, FFN_DIM), D_MODEL ** -0.5),
        "moe_w3": nrm(ks[22], (N_MOE, N_EXPERTS, D_MODEL, FFN_DIM), D_MODEL ** -0.5),
        "moe_w2": nrm(ks[23], (N_MOE, N_EXPERTS, FFN_DIM, D_MODEL), FFN_DIM ** -0.5),
    }


def reference(x, c, positions, ada_w, ada_b, norm_mix_g, norm_ffn_g, w_in,
              mla_q_norm_g, mla_kv_norm_g, mla_w_uq, mla_w_ukv, mla_q_head_g, mla_k_head_g,
              moba_q_head_g, moba_k_head_g, group_norm_g, w_out,
              ffn_w1, ffn_w3, ffn_w2, router_w, moe_w1, moe_w3, moe_w2):
    cos_pe, sin_pe = rope_tables(positions, MLA_ROPE_DIM)
    cos_full, sin_full = rope_tables(positions, HEAD_DIM)
    cond = jax.nn.silu(c)
    for l in range(DEPTH):
        mod = cond @ ada_w[l] + ada_b[l]
        shift_m, scale_m, gate_m, shift_f, scale_f, gate_f = jnp.split(mod, 6, axis=-1)
        h = modulate(rms_norm(x, norm_mix_g[l]), shift_m, scale_m)
        y = hybrid_mixer(h, w_in[l], mla_q_norm_g[l], mla_kv_norm_g[l], mla_w_uq[l], mla_w_ukv[l],
                         mla_q_head_g[l], mla_k_head_g[l], moba_q_head_g[l], moba_k_head_g[l],
                         group_norm_g[l], w_out[l], cos_pe, sin_pe, cos_full, sin_full)
        x = x + gate_m[:, None, :] * y
        h = modulate(rms_norm(x, norm_ffn_g[l]), shift_f, scale_f)
        j = l // 2
        if l % 2 == 0:
            y = swiglu(h, ffn_w1[j], ffn_w3[j], ffn_w2[j])
        else:
            y = moe_swiglu(h, router_w[j], moe_w1[j], moe_w3[j], moe_w2[j])
        x = x + gate_f[:, None, :] * y
    return x
```

```python
import math
from contextlib import ExitStack
import numpy as np
import concourse.bass as bass
import concourse.mybir as mybir
from concourse.bass_utils import run_bass_kernel_spmd

F32 = mybir.dt.float32
BF16 = mybir.dt.bfloat16
I32 = mybir.dt.int32
AF = mybir.ActivationFunctionType
ALU = mybir.AluOpType
AX = mybir.AxisListType

D = 2048
SEQ = 2048
NT = 16
FF = 5632
NFC = 44
INW = 5952
EPS = 1e-6
EPOCH = 6000
ARENA = 53000


class Sch:
    ENG = ['pe', 'act', 'dve', 'pool', 'sp']

    def __init__(self, nc, es):
        self.nc = nc
        self.es = es
        self.q = {e: [] for e in self.ENG}
        self.semh = {}
        self.cnt = {e: 0 for e in ['pe', 'act', 'dve', 'pool']}
        self.ep = {e: 0 for e in ['pe', 'act', 'dve', 'pool']}
        self.waited = {e: {} for e in self.ENG}
        self.lastw = {}
        self.readers = {}
        self.nds = 24
        self.dcnt = [0] * self.nds
        self.dnext = 0

    def sem(self, name):
        if name not in self.semh:
            self.semh[name] = self.es.enter_context(self.nc.semaphore(name))
        return self.semh[name]

    def _need(self, reads, writes, eng=None):
        need = {}

        def add(st):
            if st is not None and need.get(st[0], 0) < st[1]:
                need[st[0]] = st[1]
        for k in reads:
            add(self.lastw.get(k))
            if isinstance(k, str) and k.startswith('ps') and eng is not None:
                for s, v in self.readers.get(k, {}).items():
                    if not s.startswith(eng):
                        add((s, v))
        for k in writes:
            add(self.lastw.get(k))
            for s, v in self.readers.get(k, {}).items():
                add((s, v))
        return need

    def _waits(self, eng, need):
        for s, v in need.items():
            if eng == 'pe' and s.startswith('pe'):
                continue
            if self.waited[eng].get(s, 0) >= v:
                continue
            self.waited[eng][s] = v
            self.sem(s)
            self.q[eng].append(('w', s, v))

    def _mark(self, st, reads, writes):
        for k in writes:
            self.lastw[k] = st
            self.readers[k] = {}
        for k in reads:
            d = self.readers.setdefault(k, {})
            if d.get(st[0], 0) < st[1]:
                d[st[0]] = st[1]

    def op(self, eng, emit, reads=(), writes=(), signal=True):
        self._waits(eng, self._need(reads, writes, eng))
        if signal:
            if self.cnt[eng] >= EPOCH:
                self.ep[eng] += 1
                self.cnt[eng] = 0
            name = f"{eng}{self.ep[eng]}"
            self.sem(name)
            self.cnt[eng] += 1
            st = (name, self.cnt[eng])
            self.q[eng].append(('i', emit, name))
        else:
            if self.cnt[eng] >= EPOCH:
                st = (f"{eng}{self.ep[eng] + 1}", 1)
            else:
                st = (f"{eng}{self.ep[eng]}", self.cnt[eng] + 1)
            self.sem(st[0])
            self.q[eng].append(('n', emit))
        self._mark(st, reads, writes)

    def dma(self, eng, out, in_, reads=(), writes=()):
        i = self.dnext
        self.dnext = (i + 1) % self.nds
        name = f"dq{i}"
        self.sem(name)
        need = self._need(reads, writes)
        if self.dcnt[i] > 0:
            need[name] = 16 * self.dcnt[i]
        self._waits(eng, need)
        self.dcnt[i] += 1
        st = (name, 16 * self.dcnt[i])
        self.q[eng].append(('d', out, in_, name))
        self._mark(st, reads, writes)

    def barrier(self):
        cur = {}
        for e in self.cnt:
            if self.cnt[e] > 0:
                cur[f"{e}{self.ep[e]}"] = self.cnt[e]
        for i in range(self.nds):
            if self.dcnt[i] > 0:
                cur[f"dq{i}"] = 16 * self.dcnt[i]
        for e in self.ENG:
            self._waits(e, dict(cur))
        self.lastw.clear()
        self.readers.clear()

    def emit_all(self):
        nc = self.nc
        block = self.es.enter_context(nc.Block())
        semh = self.semh

        def run(lst):
            def f(e):
                for it in lst:
                    if it[0] == 'w':
                        e.wait_ge(semh[it[1]], it[2])
                    elif it[0] == 'i':
                        it[1](e).then_inc(semh[it[2]], 1)
                    elif it[0] == 'n':
                        it[1](e)
                    else:
                        e.dma_start(out=it[1], in_=it[2]).then_inc(semh[it[3]], 16)
            return f
        block.tensor(run(self.q['pe']))
        block.scalar(run(self.q['act']))
        block.vector(run(self.q['dve']))
        block.gpsimd(run(self.q['pool']))
        block.sync(run(self.q['sp']))


class Arena:
    def __init__(self, ap):
        self.ap = ap
        self.off = 0
        self.marks = []

    def push(self):
        self.marks.append(self.off)

    def pop(self):
        self.off = self.marks.pop()

    def f32(self, n):
        o = self.off
        self.off += n
        assert self.off <= ARENA, f"arena overflow {self.off}"
        return self.ap[:, o:o + n]

    def bf16(self, n):
        nf = (n + 1) // 2
        return self.f32(nf).bitcast(BF16)[:, 0:n]


def host_consts():
    c = {}
    c['ident'] = np.eye(128, dtype=np.float32)
    ki = np.arange(128)[:, None]
    xx = np.arange(512)[None, :]
    c['cmask'] = (xx >= ki).astype(np.float32)
    c['smask'] = (xx > ki).astype(np.float32)
    jj = np.arange(128)[:, None]
    kk = np.arange(128)[None, :]
    c['umat'] = (jj > kk).astype(np.float32)
    c['ones'] = np.ones((128, 128), np.float32)
    inv_pe = 10000.0 ** (-np.arange(0, 64, 2, dtype=np.float32) / 64)
    inv_full = 10000.0 ** (-np.arange(0, 128, 2, dtype=np.float32) / 128)
    c['invf'] = np.tile(np.concatenate([inv_pe, inv_full])[None, :].astype(np.float32), (128, 1))
    rett = np.zeros((128, 4, 2, 512), np.float32)
    for h in range(4):
        lg = math.log(1.0 - 2.0 ** (-5.0 - h))
        e = (xx - ki).astype(np.float64)
        rett[:, h, 0, :] = np.exp(e * lg)
        rett[:, h, 1, :] = np.where(e >= 0, np.exp(np.maximum(e, 0) * lg), 0.0)
    c['rett'] = rett.reshape(128, 4096)
    sel8 = np.zeros((8, 8, 128), np.float32)
    for e in range(8):
        sel8[e, e, :] = 1.0
    c['sel8'] = sel8.reshape(8, 1024)
    mc = np.zeros((128, 3, 16, 8), np.float32)
    for t in range(16):
        cur = t // 2
        for n in range(8):
            mc[:, 0, t, n] = 1.0 if n < cur else 0.0
            mc[:, 1, t, n] = 0.0 if n < cur else -1e30
            mc[:, 2, t, n] = 1.0 if n == cur else 0.0
    c['mobac'] = mc.reshape(128, 384)
    return c


CONST_SHAPES = {'ident': [128, 128], 'cmask': [128, 512], 'smask': [128, 512], 'umat': [128, 128],
                'ones': [128, 128], 'invf': [128, 96], 'rett': [128, 4096], 'sel8': [8, 1024], 'mobac': [128, 384]}

NCOL = 64 + 16 + 16 + 4 + 2 + 16
NROW = 640

WEIGHTS = [("ada_w", [2, D, 6 * D]), ("w_in", [2, D, INW]), ("mla_w_uq", [2, 512, 768]), ("mla_w_ukv", [2, 256, 1024]),
           ("w_out", [2, D, D]), ("ffn_w1", [1, D, FF]), ("ffn_w3", [1, D, FF]), ("ffn_w2", [1, FF, D]),
           ("router_w", [1, D, 8]), ("moe_w1", [1, 8, D, FF]), ("moe_w3", [1, 8, D, FF]), ("moe_w2", [1, 8, FF, D])]

GAMMA = [1.0 - 2.0 ** (-5.0 - h) for h in range(4)]


def build(stop_after=None, debug=False):
    nc = bass.Bass("TRN2", target_bir_lowering=False)
    dr = {}

    def din(name, shape, dt=F32):
        dr[name] = nc.dram_tensor(name, shape, dt, kind="ExternalInput").ap()
        return dr[name]

    din("x", [SEQ, D])
    din("c_col", [128, 16])
    din("pos", [128, 16], I32)
    din("colp", [128, 2 * NCOL])
    din("rowp", [128, 2 * NROW])
    din("adabr", [128, 2 * 2 * D])
    for k, shp in CONST_SHAPES.items():
        din(k, shp)
    nst_ = 4 if stop_after is None else stop_after
    for k, shp in WEIGHTS:
        if k.startswith('ffn') and nst_ < 2:
            continue
        if (k.startswith('moe') or k.startswith('router')) and nst_ < 4:
            continue
        din(k, shp)
    dbg_outs = {}

    def dbg_dump(S, name, ap, shape, dt, keys):
        if not debug:
            return
        t = nc.dram_tensor(name, shape, dt, kind="ExternalOutput").ap()
        S.dma('sp', t, ap, keys, [f'dbg_{name}'])
    out_d = nc.dram_tensor("out", [SEQ, D], F32, kind="ExternalOutput").ap()
    skind = "ExternalOutput" if debug else "Internal"
    ycat_d = nc.dram_tensor("ycat_raw", [SEQ, D], F32, kind=skind).ap()
    x1_d = nc.dram_tensor("x1s", [SEQ, D], F32, kind=skind).ap()
    x2_d = nc.dram_tensor("x2s", [SEQ, D], F32, kind=skind).ap()
    gate_d = nc.dram_tensor("gaterow", [128, 4 * D], F32, kind=skind).ap()

    es = ExitStack()
    with es:
        arena_t = es.enter_context(nc.sbuf_tensor("arena", [128, ARENA], F32))
        A = Arena(arena_t)
        PS = [es.enter_context(nc.psum_tensor(f"ps{i}", [128, 512], F32))[:, :] for i in range(8)]
        S = Sch(nc, es)
        pk = [f"ps{i}" for i in range(8)]

        def act(out, in_, func, reads, writes, **kw):
            S.op('act', lambda e: e.activation(out=out, in_=in_, func=func, **kw), reads, writes)

        def ts(out, in0, s1, s2, op0, op1, reads, writes, eng='dve'):
            if op1 is None:
                S.op(eng, lambda e: e.tensor_scalar(out=out, in0=in0, scalar1=s1, scalar2=None, op0=op0), reads, writes)
            else:
                S.op(eng, lambda e: e.tensor_scalar(out=out, in0=in0, scalar1=s1, scalar2=s2, op0=op0, op1=op1), reads, writes)

        def tt(out, in0, in1, op, reads, writes, eng='dve'):
            S.op(eng, lambda e: e.tensor_tensor(out=out, in0=in0, in1=in1, op=op), reads, writes)

        def stt(out, in0, sc, in1, op0, op1, reads, writes, eng='dve'):
            S.op(eng, lambda e: e.scalar_tensor_tensor(out=out, in0=in0, scalar=sc, in1=in1, op0=op0, op1=op1), reads, writes)

        def cp(out, in_, reads, writes, eng='dve'):
            S.op(eng, lambda e: e.tensor_copy(out=out, in_=in_), reads, writes)

        def mset(ap, val, writes, eng='dve'):
            S.op(eng, lambda e: e.memset(ap, val), (), writes)

        def mm(out, lhsT, rhs, start, stop, reads, writes, sig=None):
            S.op('pe', lambda e: e.matmul(out, lhsT=lhsT, rhs=rhs, start=start, stop=stop), reads, writes,
                 signal=(stop if sig is None else sig))

        def tr(out, in_, reads, writes):
            S.op('pe', lambda e: e.transpose(out, in_, ident), list(reads) + ['const'], writes)

        def rstd_from_ss(rs, ss, n, keys):
            act(rs, ss, AF.Ln, keys, keys, scale=1.0 / n, bias=EPS)
            act(rs, rs, AF.Exp, keys, keys, scale=-0.5)

        ident = A.f32(128)
        colp = A.f32(2 * NCOL)
        modc = A.f32(2 * 64)
        S.dma('sp', ident, dr['ident'], (), ['const'])
        S.dma('sp', colp, dr['colp'], (), ['const'])

        def phase_ada():
            A.push()
            ones = A.f32(128)
            ccol = A.f32(16)
            cond = A.f32(16)
            condrep = A.f32(16 * 128).rearrange("p (k m) -> p k m", m=128)
            wb = [A.f32(16 * 512).rearrange("p (k n) -> p k n", n=512) for _ in range(2)]
            brow = A.f32(512)
            orow = [A.f32(512) for _ in range(2)]
            mtmp = A.f32(64)
            S.dma('sp', ones, dr['ones'], (), ['ones'])
            S.dma('sp', ccol, dr['c_col'], (), ['ccol'])
            act(cond, ccol, AF.Silu, ['ccol'], ['cond'])
            for kc in range(16):
                ts(condrep[:, kc, :], ones, cond[:, kc:kc + 1], None, ALU.mult, None, ['ones', 'cond'], [f'crep{kc}'])
            wi = 0
            for l in range(2):
                awv = dr['ada_w'][l].rearrange("(kc p) n -> p kc n", p=128)
                for gi_i, gi in enumerate([0, 1, 3, 4]):
                    for jb in range(4):
                        w = wb[wi % 2]
                        wk = f'adaw{wi % 2}'
                        wi += 1
                        S.dma('sp', w, awv[:, :, gi * D + jb * 512: gi * D + (jb + 1) * 512], (), [wk])
                        for kc in range(16):
                            mm(PS[1], condrep[:, kc, :], w[:, kc, :], kc == 0, kc == 15, [wk, f'crep{kc}'], [pk[1]])
                        o = orow[jb % 2]
                        ok_ = f'orow{jb % 2}'
                        cp(o, PS[1], [pk[1]], [ok_])
                        for j in range(4):
                            tr(PS[2][:, j * 128:(j + 1) * 128], o[:, j * 128:(j + 1) * 128], [ok_], [pk[2]])
                        col = gi_i * 16 + jb * 4
                        cp(mtmp[:, col:col + 4], PS[2].rearrange("p (j m) -> p j m", m=128)[:, :, 0], [pk[2]], ['mtmp'])
                mraw = modc[:, l * 64:(l + 1) * 64]
                tt(mraw, mtmp, colp[:, l * NCOL:l * NCOL + 64], ALU.add, ['mtmp', 'const'], [f'modc{l}'])
                gm = colp[:, l * NCOL + 64:l * NCOL + 80]
                gf = colp[:, l * NCOL + 80:l * NCOL + 96]
                stt(mraw[:, 16:32], mraw[:, 16:32], 1.0, gm, ALU.add, ALU.mult, [f'modc{l}', 'const'], [f'modc{l}'])
                stt(mraw[:, 48:64], mraw[:, 48:64], 1.0, gf, ALU.add, ALU.mult, [f'modc{l}', 'const'], [f'modc{l}'])
                for g_i, gi in enumerate([2, 5]):
                    for jb in range(4):
                        w = wb[wi % 2]
                        wk = f'adaw{wi % 2}'
                        wi += 1
                        S.dma('sp', w, awv[:, :, gi * D + jb * 512: gi * D + (jb + 1) * 512], (), [wk])
                        for kc in range(16):
                            mm(PS[1], condrep[:, kc, :], w[:, kc, :], kc == 0, kc == 15, [wk, f'crep{kc}'], [pk[1]])
                        c0 = (l * 2 + g_i) * D + jb * 512
                        S.dma('sp', brow, dr['adabr'][:, c0:c0 + 512], (), ['brow'])
                        o = orow[(jb) % 2]
                        ok_ = f'orow{jb % 2}'
                        tt(o, PS[1], brow, ALU.add, [pk[1], 'brow'], [ok_])
                        S.dma('sp', gate_d[:, c0:c0 + 512], o, [ok_], [f'gated{c0}'])
            S.barrier()
            A.pop()

        def mod_cols(l, which):
            base = l * 64 + (0 if which == 'm' else 32)
            return modc[:, base + 16:base + 32], modc[:, base:base + 16]

        def norm_T(src_d, dstT, dst_key, t0, t1, ngroups, acol, bcol, xt_bufs, junk, stat, hook=None):
            gw = D // ngroups
            for t in range(t0, t1):
                xt = xt_bufs[t % 2]
                xk = f'xt{t % 2}'
                S.dma('sp', xt, src_d[t * 128:(t + 1) * 128, :], (), [xk])
                ss = stat[:, 0:ngroups]
                rs = stat[:, 4:4 + ngroups]
                for g in range(ngroups):
                    act(junk[:, 0:gw], xt[:, g * gw:(g + 1) * gw], AF.Square, [xk], ['nstat'], accum_out=ss[:, g:g + 1])
                rstd_from_ss(rs, ss, gw, ['nstat'])
                for g in range(ngroups):
                    act(xt[:, g * gw:(g + 1) * gw], xt[:, g * gw:(g + 1) * gw], AF.Identity, [xk, 'nstat'], [xk], scale=rs[:, g:g + 1])
                for q4 in range(4):
                    p = PS[6 + (q4 % 2)]
                    pkk = pk[6 + (q4 % 2)]
                    for j in range(4):
                        dc = q4 * 4 + j
                        tr(p[:, j * 128:(j + 1) * 128], xt[:, dc * 128:(dc + 1) * 128], [xk], [pkk])
                    for j in range(4):
                        dc = q4 * 4 + j
                        o = dstT[:, dc, (t - t0) * 128:(t - t0 + 1) * 128]
                        if bcol is None:
                            act(o, p[:, j * 128:(j + 1) * 128], AF.Identity, [pkk, 'const', 'modc0', 'modc1'], [f'{dst_key}{t}'],
                                scale=acol[:, dc:dc + 1])
                        else:
                            act(o, p[:, j * 128:(j + 1) * 128], AF.Identity, [pkk, 'const', 'modc0', 'modc1'], [f'{dst_key}{t}'],
                                scale=acol[:, dc:dc + 1], bias=bcol[:, dc:dc + 1])
                        if hook is not None:
                            hook(t, dc, p[:, j * 128:(j + 1) * 128], pkk)

        wctr = [0]

        def load_w(bufs, key, src_ap, kcn, ncols):
            i = wctr[0] % len(bufs)
            wctr[0] += 1
            w = bufs[i][:, 0:kcn, 0:ncols]
            S.dma('pool', w, src_ap.rearrange("(kc p) n -> p kc n", p=128), (), [f'{key}{i}'])
            return w, f'{key}{i}'

        def phase_mixer(l, src_d):
            A.push()
            win = dr['w_in'][l]
            hT = A.bf16(16 * SEQ).rearrange("p (k t) -> p k t", t=SEQ)
            cmask = A.f32(512)
            smask = A.f32(512)
            umat = A.f32(128)
            ones = A.f32(128)
            invf = A.f32(96)
            rowp = A.f32(NROW)
            posi = A.f32(16).bitcast(I32)
            posf = A.f32(16)
            cs_pe = A.f32(2 * 16 * 32).rearrange("p (c t d) -> p c t d", c=2, t=16)
            cs_full = A.f32(2 * 16 * 64).rearrange("p (c t d) -> p c t d", c=2, t=16)
            wbufs = [A.bf16(16 * 256).rearrange("p (k n) -> p k n", n=256) for _ in range(2)]
            xt_bufs = [A.f32(D) for _ in range(2)]
            junk = A.bf16(D)
            stat = A.f32(16)
            for nm, ap_ in [('cmask', cmask), ('smask', smask), ('umat', umat), ('ones', ones), ('invf', invf)]:
                S.dma('sp', ap_, dr[nm], (), ['mconst'])
            S.dma('sp', rowp, dr['rowp'][:, l * NROW:(l + 1) * NROW], (), ['mconst'])
            S.dma('sp', posi, dr['pos'], (), ['posi'])
            cp(posf, posi, ['posi'], ['posf'])
            tmp_ang = xt_bufs[0]
            for (tab, i0, hd) in [(cs_pe, 0, 32), (cs_full, 32, 64)]:
                ang = tmp_ang[:, 0:16 * hd].rearrange("p (t d) -> p t d", d=hd)
                for t in range(16):
                    ts(ang[:, t, :], invf[:, i0:i0 + hd], posf[:, t:t + 1], None, ALU.mult, None, ['mconst', 'posf'], ['xt0'])
                for ci, shift in [(1, 0.0), (0, 0.25)]:
                    a2 = xt_bufs[1][:, 0:16 * hd].rearrange("p (t d) -> p t d", d=hd)
                    a3 = xt_bufs[1][:, 1024:1024 + 16 * hd].rearrange("p (t d) -> p t d", d=hd)
                    a3i = xt_bufs[1][:, 1024:1024 + 16 * hd].bitcast(I32).rearrange("p (t d) -> p t d", d=hd)
                    ts(a2, ang, 1.0 / (2 * math.pi), shift, ALU.mult, ALU.add, ['xt0'], ['xt1'])
                    cp(a3i, a2, ['xt1'], ['xt1'])
                    cp(a3, a3i, ['xt1'], ['xt1'])
                    tt(a2, a2, a3, ALU.subtract, ['xt1', 'xt1'], ['xt1'])
                    ts(a3, a2, 0.5, None, ALU.is_gt, None, ['xt1'], ['xt1'])
                    tt(a2, a2, a3, ALU.subtract, ['xt1', 'xt1'], ['xt1'])
                    act(tab[:, ci], a2, AF.Sin, ['xt1'], ['rope'], scale=2 * math.pi)
            am, bm = mod_cols(l, 'm')
            norm_T(src_d, hT, 'hT', 0, 16, 1, am, bm, xt_bufs, junk, stat)
            hkeys = [f'hT{t}' for t in range(16)]
            if l == 0:
                dbg_dump(S, 'd_hT', hT.rearrange("p k t -> p (k t)"), [128, 16 * SEQ], BF16, hkeys)

            ycv = ycat_d.rearrange("(t p) c -> p t c", p=128)

            def proj_tm(col0, ncols, cb):
                if ncols < 256:
                    w, wk = load_w(wbufs, 'win', win[:, col0:col0 + 256], 16, 256)
                    w = w[:, :, 0:ncols]
                else:
                    w, wk = load_w(wbufs, 'win', win[:, col0:col0 + ncols], 16, ncols)
                def grp(t):
                    pi = 6 + (t % 2)
                    for kc in range(16):
                        mm(PS[pi][:, 0:ncols], hT[:, kc, t * 128:(t + 1) * 128], w[:, kc, :], kc == 0, kc == 15,
                           [wk, f'hT{t}'], [pk[pi]])
                grp(0)
                pending = None
                for t in range(16):
                    if t + 1 < 16:
                        grp(t + 1)
                    pi = 6 + (t % 2)
                    fin2 = cb(t, PS[pi][:, 0:ncols], pk[pi])
                    if pending is not None:
                        pending()
                    pending = fin2
                if pending is not None:
                    pending()

            def proj_fm(col0, ncols, cb):
                w, wk = load_w(wbufs, 'win', win[:, col0:col0 + ncols], 16, ncols)
                for j in range(ncols // 128):
                    for tb in range(4):
                        pi = 6 + (tb % 2)
                        for kc in range(16):
                            mm(PS[pi], w[:, kc, j * 128:(j + 1) * 128], hT[:, kc, tb * 512:(tb + 1) * 512], kc == 0, kc == 15,
                               [wk] + hkeys[tb * 4:tb * 4 + 4], [pk[pi]])
                        cb(j, tb, PS[pi], pk[pi])

            def rope_tm(dst, src, skeys, tab, t, hd, tmpa, tmpb):
                cos = tab[:, 0, t, :]
                sin = tab[:, 1, t, :]
                x1 = src[:, 0:hd]
                x2 = src[:, hd:2 * hd]
                tt(tmpa[:, 0:hd], x1, cos, ALU.mult, skeys + ['rope'], ['ropetmp'])
                tt(tmpb[:, 0:hd], x2, sin, ALU.mult, skeys + ['rope'], ['ropetmp2'])
                tt(tmpa[:, hd:2 * hd], x2, cos, ALU.mult, skeys + ['rope'], ['ropetmp'])
                tt(tmpb[:, hd:2 * hd], x1, sin, ALU.mult, skeys + ['rope'], ['ropetmp2'])
                tt(dst[:, 0:hd], tmpa[:, 0:hd], tmpb[:, 0:hd], ALU.subtract, ['ropetmp', 'ropetmp2'], skeys)
                tt(dst[:, hd:2 * hd], tmpa[:, hd:2 * hd], tmpb[:, hd:2 * hd], ALU.add, ['ropetmp', 'ropetmp2'], skeys)

            def attn_core(nd, qT, kT, qkeys, kkeys, vt, vkeys, dvx, score_fn, descending, finish_fn):
                pairs = []
                for qb in range(4):
                    kts = list(range(0, 4 * qb + 4))
                    if descending:
                        kts = kts[::-1]
                    for idx, kt in enumerate(kts):
                        pairs.append((qb, kt, idx == len(kts) - 1))
                accs = [PS[4 + qs][:, 0:dvx] for qs in range(4)]
                acck = [pk[4 + qs] for qs in range(4)]

                def geom(i):
                    qb, kt, _ = pairs[i]
                    j = kt - 4 * qb
                    c0 = 128 * max(j, 0)
                    return qb, kt, j, c0, 512 - c0

                def emit_s(i):
                    qb, kt, j, c0, N = geom(i)
                    si = i % 2
                    ps_s = PS[si][:, 0:N]
                    for di in range(nd):
                        qa, kp = qT[di]
                        ka, _ = kT[di]
                        mm(ps_s, ka[0:kp, kt * 128:(kt + 1) * 128], qa[0:kp, qb * 512 + c0:(qb + 1) * 512], di == 0, di == nd - 1,
                           qkeys + kkeys, [pk[si]])
                emit_s(0)
                for i in range(len(pairs)):
                    if i + 1 < len(pairs):
                        emit_s(i + 1)
                    qb, kt, j, c0, N = geom(i)
                    si = i % 2
                    PT = PTb[si]
                    ptk = f'PT{si}'
                    score_fn(qb, kt, j, c0, N, PS[si][:, 0:N], pk[si], PT, ptk)
                    for qs in range(max(j, 0), 4):
                        first = (kt == 4 * qb + qs) if descending else (kt == 0)
                        last = (kt == 0) if descending else (kt == 4 * qb + qs)
                        mm(accs[qs], PT[:, qs * 128:(qs + 1) * 128], vt(kt), first, last, [ptk] + vkeys, [acck[qs]], sig=True)
                    if pairs[i][2]:
                        finish_fn(qb, accs, acck)

            PTb = [A.bf16(512) for _ in range(2)]
            yout = [A.f32(512).rearrange("p (a b) -> p a b", b=128) for _ in range(2)]
            youtc = [0]

            def store_y(qb, col0, fill):
                i = youtc[0] % 2
                youtc[0] += 1
                yo = yout[i]
                fill(yo, f'yout{i}')
                S.dma('sp', ycv[:, 4 * qb:4 * qb + 4, col0:col0 + 128], yo, [f'yout{i}'], [f'ycat{qb}_{col0}'])

            mixer_base = A.off

            def mixer_sb():
                A.off = mixer_base
                QT = A.bf16(4 * SEQ).rearrange("p (h t) -> p h t", t=SEQ)
                KT = A.bf16(4 * SEQ).rearrange("p (h t) -> p h t", t=SEQ)
                V = A.bf16(16 * 512).rearrange("p (t c) -> p t c", c=512)
                t_e = A.f32(512)
                t_sp = A.f32(512)
                t_L = A.f32(512)
                t_1 = A.f32(512)
                carry = A.f32(512)
                for (dst, c0, nm) in [(QT, 2880, 'sbq'), (KT, 3392, 'sbk')]:
                    for half in range(2):
                        def cb(j, tb, ps, pkey, dst=dst, half=half, nm=nm):
                            act(dst[:, half * 2 + j, tb * 512:(tb + 1) * 512], ps, AF.Identity, [pkey], [f'{nm}{half * 2 + j}'])
                        proj_fm(c0 + half * 256, 256, cb)
                for half in range(2):
                    def cbv(t, ps, pkey, half=half):
                        act(V[:, t, half * 256:(half + 1) * 256], ps, AF.Identity, [pkey], ['sbv'])
                    proj_tm(3904 + half * 256, 256, cbv)
                scale = 128 ** -0.5
                if l == 0:
                    dbg_dump(S, 'd_sbq', QT.rearrange("p h t -> p (h t)"), [128, 4 * SEQ], BF16, [f'sbq{i}' for i in range(4)])
                    dbg_dump(S, 'd_sbk', KT.rearrange("p h t -> p (h t)"), [128, 4 * SEQ], BF16, [f'sbk{i}' for i in range(4)])
                    dbg_dump(S, 'd_sbv', V.rearrange("p t c -> p (t c)"), [128, 16 * 512], BF16, ['sbv'])
                for h in range(4):
                    def score(qb, kt, j, c0, N, ps_s, psk, PT, ptk):
                        diag = j >= 0
                        first = (j == 3)
                        if first:
                            mset(carry, 0.0, ['carry'])
                        act(t_e[:, 0:N], ps_s, AF.Exp, [psk], ['t_e'], scale=scale)
                        act(t_sp[:, 0:N], t_e[:, 0:N], AF.Ln, ['t_e'], ['t_sp'], bias=1.0)
                        if diag:
                            stt(t_L[:, 0:N], t_sp[:, 0:N], -1.0, smask[:, 0:N], ALU.mult, ALU.mult, ['t_sp', 'mconst'], ['t_L'])
                        else:
                            ts(t_L[:, 0:N], t_sp[:, 0:N], -1.0, None, ALU.mult, None, ['t_sp'], ['t_L'])
                        mm(PS[2][:, 0:N], umat, t_L[:, 0:N], True, True, ['mconst', 't_L'], [pk[2]])
                        mm(PS[3][:, 0:N], ones, t_L[:, 0:N], True, True, ['mconst', 't_L'], [pk[3]])
                        stt(t_1[:, 0:N], ps_s, scale, t_sp[:, 0:N], ALU.mult, ALU.subtract, [psk, 't_sp'], ['t_1'])
                        tt(t_1[:, 0:N], t_1[:, 0:N], PS[2][:, 0:N], ALU.add, ['t_1', pk[2]], ['t_1'])
                        tt(t_1[:, 0:N], t_1[:, 0:N], carry[:, c0:512], ALU.add, ['t_1', 'carry'], ['t_1'])
                        if diag:
                            act(t_e[:, 0:N], t_1[:, 0:N], AF.Exp, ['t_1'], ['t_e'])
                            tt(PT[:, c0:512], t_e[:, 0:N], smask[:, 0:N], ALU.mult, ['t_e', 'mconst'], [ptk])
                        else:
                            act(PT[:, c0:512], t_1[:, 0:N], AF.Exp, ['t_1'], [ptk])
                        tt(carry[:, c0:512], carry[:, c0:512], PS[3][:, 0:N], ALU.add, ['carry', pk[3]], ['carry'])

                    def fin(qb, accs, acck, h=h):
                        def fill(yo, key):
                            for qs in range(4):
                                act(yo[:, qs, :], accs[qs], AF.Identity, [acck[qs]], [key])
                        store_y(qb, 1024 + h * 128, fill)
                    attn_core(1, [(QT[:, h, :], 128)], [(KT[:, h, :], 128)], [f'sbq{h}'], [f'sbk{h}'],
                              lambda kt, h=h: V[:, kt, h * 128:(h + 1) * 128], ['sbv'], 128, score, True, fin)

            def mixer_ret():
                A.off = mixer_base
                rett1 = A.f32(1024).rearrange("p (m x) -> p m x", m=2)
                QT = A.bf16(4 * SEQ).rearrange("p (h t) -> p h t", t=SEQ)
                KT = A.bf16(4 * SEQ).rearrange("p (h t) -> p h t", t=SEQ)
                V = A.bf16(16 * 512).rearrange("p (t c) -> p t c", c=512)
                SG = A.bf16(16 * 512).rearrange("p (t c) -> p t c", c=512)
                rt2 = [A.f32(256) for _ in range(2)]
                ra = A.f32(128)
                rb = A.f32(128)
                gst = A.f32(16)
                yc = A.f32(512).rearrange("p (a b) -> p a b", b=128)
                sgf = A.f32(512).rearrange("p (a b) -> p a b", b=128)
                for (dst, c0, nm) in [(QT, 832, 'rq'), (KT, 1344, 'rk')]:
                    for half in range(2):
                        def cb(t, ps, pkey, dst=dst, half=half, nm=nm):
                            rtt = rt2[t % 2]
                            rk = f'rt{t % 2}'
                            for hh in range(2):
                                rope_tm(rtt[:, hh * 128:(hh + 1) * 128], ps[:, hh * 128:(hh + 1) * 128], [pkey, rk], cs_full, t, 64, ra, rb)
                            p = PS[2 + (t % 2)]
                            for hh in range(2):
                                tr(p[:, hh * 128:(hh + 1) * 128], rtt[:, hh * 128:(hh + 1) * 128], [rk], [pk[2 + (t % 2)]])

                            def st2():
                                for hh in range(2):
                                    h = half * 2 + hh
                                    act(dst[:, h, t * 128:(t + 1) * 128], p[:, hh * 128:(hh + 1) * 128], AF.Identity, [pk[2 + (t % 2)]], [f'{nm}{h}'])
                            return st2
                        proj_tm(c0 + half * 256, 256, cb)
                for half in range(2):
                    def cbv(t, ps, pkey, half=half):
                        act(V[:, t, half * 256:(half + 1) * 256], ps, AF.Identity, [pkey], ['rv'])
                    proj_tm(1856 + half * 256, 256, cbv)
                for half in range(2):
                    def cbg(t, ps, pkey, half=half):
                        act(SG[:, t, half * 256:(half + 1) * 256], ps, AF.Silu, [pkey], ['rg'])
                    proj_tm(2368 + half * 256, 256, cbg)
                scale = 128 ** -0.5
                for h in range(4):
                    S.dma('sp', rett1.rearrange("p m x -> p (m x)"), dr['rett'][:, h * 1024:(h + 1) * 1024], (), ['rett'])

                    def score(qb, kt, j, c0, N, ps_s, psk, PT, ptk, h=h):
                        if j >= 0:
                            stt(PT[:, c0:512], ps_s, scale, rett1[:, 1, 0:N], ALU.mult, ALU.mult, [psk, 'rett'], [ptk])
                        else:
                            dl = 512 * qb - 128 * kt
                            stt(PT[:, c0:512], ps_s, scale * (GAMMA[h] ** dl), rett1[:, 0, 0:N], ALU.mult, ALU.mult, [psk, 'rett'], [ptk])

                    def fin(qb, accs, acck, h=h):
                        def fill(yo, key):
                            for qs in range(4):
                                act(yc[:, qs, :], accs[qs], AF.Identity, [acck[qs]], ['yc'], accum_out=gst[:, qs:qs + 1])
                            ts(gst[:, 4:8], gst[:, 0:4], 1.0 / 128, None, ALU.mult, None, ['yc'], ['yc'])
                            for qs in range(4):
                                ts(yc[:, qs, :], yc[:, qs, :], gst[:, 4 + qs:5 + qs], None, ALU.subtract, None, ['yc'], ['yc'])
                            for qs in range(4):
                                act(sgf[:, qs, :], yc[:, qs, :], AF.Square, ['yc'], ['sgf'], accum_out=gst[:, 8 + qs:9 + qs])
                            rstd_from_ss(gst[:, 12:16], gst[:, 8:12], 128, ['sgf', 'yc'])
                            for qs in range(4):
                                t = 4 * qb + qs
                                stt(yo[:, qs, :], yc[:, qs, :], gst[:, 12 + qs:13 + qs], SG[:, t, h * 128:(h + 1) * 128], ALU.mult, ALU.mult,
                                    ['yc', 'sgf', 'rg'], [key])
                        store_y(qb, 512 + h * 128, fill)
                    attn_core(1, [(QT[:, h, :], 128)], [(KT[:, h, :], 128)], [f'rq{h}'], [f'rk{h}'],
                              lambda kt, h=h: V[:, kt, h * 128:(h + 1) * 128], ['rv'], 128, score, False, fin)

            def mixer_moba():
                A.off = mixer_base
                mobac = A.f32(384).rearrange("p (m t n) -> p m t n", m=3, t=16)
                sel8 = A.f32(1024).rearrange("p (e m) -> p e m", m=128)
                QT = A.bf16(2 * SEQ).rearrange("p (h t) -> p h t", t=SEQ)
                KT = A.bf16(2 * SEQ).rearrange("p (h t) -> p h t", t=SEQ)
                V = A.bf16(16 * 2 * 130).rearrange("p (t h c) -> p t h c", t=16, h=2)
                selB = A.bf16(8 * SEQ).rearrange("p (n t) -> p n t", t=SEQ)
                selall = A.f32(128).rearrange("p (t n) -> p t n", n=8)
                selT = A.f32(SEQ)
                km = A.f32(8)
                kmb = A.bf16(8)
                gm = A.f32(8)
                top8 = A.f32(8)
                rt2 = [A.f32(256) for _ in range(2)]
                ra = A.f32(128)
                rb = A.f32(128)
                nst = A.f32(8)
                nstc = A.f32(8)
                t_e = A.f32(512)
                S.dma('sp', mobac.rearrange("p m t n -> p (m t n)"), dr['mobac'], (), ['mobac'])
                S.dma('sp', sel8[0:8].rearrange("p e m -> p (e m)"), dr['sel8'], (), ['sel8'])
                scale = 128 ** -0.5
                for half in range(2):
                    for hh in range(2):
                        mset(V[:, :, hh, 128:129], 1.0, ['mbv'])
                    for (dst, c0, nm, goff) in [(QT, 4416, 'mq', 384), (KT, 4928, 'mk', 512)]:
                        grow = rowp[:, goff:goff + 128]

                        def cb(t, ps, pkey, dst=dst, nm=nm, grow=grow):
                            rtt = rt2[t % 2]
                            rk = f'rt{t % 2}'
                            nk_ = f'nstc{t % 2}'
                            nc4 = nstc[:, (t % 2) * 4:(t % 2) * 4 + 4]
                            for hh in range(2):
                                sl = slice(hh * 128, (hh + 1) * 128)
                                act(junk[:, 0:128], ps[:, sl], AF.Square, [pkey], [nk_], accum_out=nc4[:, hh:hh + 1])
                            rstd_from_ss(nc4[:, 2:4], nc4[:, 0:2], 128, [nk_])
                            for hh in range(2):
                                sl = slice(hh * 128, (hh + 1) * 128)
                                stt(rtt[:, sl], ps[:, sl], nc4[:, 2 + hh:3 + hh], grow, ALU.mult, ALU.mult, [pkey, nk_, 'mconst'], [rk])
                                rope_tm(rtt[:, sl], rtt[:, sl], [rk], cs_full, t, 64, ra, rb)
                            p = PS[2 + (t % 2)]
                            for hh in range(2):
                                tr(p[:, hh * 128:(hh + 1) * 128], rtt[:, hh * 128:(hh + 1) * 128], [rk], [pk[2 + (t % 2)]])

                            def st2():
                                for hh in range(2):
                                    act(dst[:, hh, t * 128:(t + 1) * 128], p[:, hh * 128:(hh + 1) * 128], AF.Identity, [pk[2 + (t % 2)]], [f'{nm}{hh}'])
                            return st2
                        proj_tm(c0 + half * 256, 256, cb)

                    def cbv(t, ps, pkey):
                        for hh in range(2):
                            act(V[:, t, hh, 0:128], ps[:, hh * 128:(hh + 1) * 128], AF.Identity, [pkey], ['mbv'])
                    proj_tm(5440 + half * 256, 256, cbv)
                    for hh in range(2):
                        h = half * 2 + hh
                        S.op('dve', lambda e, hh=hh: e.tensor_reduce(out=km, in_=KT[:, hh, :].rearrange("p (n k) -> p n k", k=256),
                                                                      axis=AX.X, op=ALU.add), [f'mk{hh}'], ['km'])
                        ts(kmb, km, 1.0 / 256, None, ALU.mult, None, ['km'], ['kmb'])
                        mm(PS[2][:, 0:8], QT[:, hh, 0:128], kmb, True, True, [f'mq{hh}', 'kmb'], [pk[2]])
                        for t in range(16):
                            pg = PS[2 + (t % 2)]
                            if t + 1 < 16:
                                mm(PS[2 + ((t + 1) % 2)][:, 0:8], QT[:, hh, (t + 1) * 128:(t + 2) * 128], kmb, True, True,
                                   [f'mq{hh}', 'kmb'], [pk[2 + ((t + 1) % 2)]])
                            tt(gm, pg[:, 0:8], mobac[:, 0, t, :], ALU.mult, [pk[2 + (t % 2)], 'mobac'], ['gm'])
                            tt(gm, gm, mobac[:, 1, t, :], ALU.add, ['gm', 'mobac'], ['gm'])
                            S.op('dve', lambda e: e.max(out=top8, in_=gm), ['gm'], ['top8'])
                            ts(selall[:, t, :], gm, top8[:, 2:3], None, ALU.is_ge, None, ['gm', 'top8'], ['selall'])
                            tt(selall[:, t, :], selall[:, t, :], mobac[:, 0, t, :], ALU.mult, ['selall', 'mobac'], ['selall'])
                            tt(selall[:, t, :], selall[:, t, :], mobac[:, 2, t, :], ALU.max, ['selall', 'mobac'], ['selall'])
                            tr(pg[0:8, 128:256], selall[:, t, :], ['selall'], [pk[2 + (t % 2)]])
                            cp(selT[0:8, t * 128:(t + 1) * 128], pg[0:8, 128:256], [pk[2 + (t % 2)]], ['selT'])
                        for n in range(8):
                            for qb in range(n // 2, 4):
                                pi = 2 + ((n * 4 + qb) % 2)
                                mm(PS[pi], sel8[0:8, n, :], selT[0:8, qb * 512:(qb + 1) * 512], True, True, ['sel8', 'selT'], [pk[pi]])
                                act(selB[:, n, qb * 512:(qb + 1) * 512], PS[pi], AF.Identity, [pk[pi]], ['selB'])

                        def score(qb, kt, j, c0, N, ps_s, psk, PT, ptk):
                            n = kt // 2
                            act(t_e[:, 0:N], ps_s, AF.Exp, [psk], ['t_e'], scale=scale)
                            if j >= 0:
                                tt(t_e[:, 0:N], t_e[:, 0:N], cmask[:, 0:N], ALU.mult, ['t_e', 'mconst'], ['t_e'])
                            tt(PT[:, c0:512], t_e[:, 0:N], selB[:, n, qb * 512 + c0:(qb + 1) * 512], ALU.mult, ['t_e', 'selB'], [ptk])

                        def fin(qb, accs, acck, h=h):
                            def fill(yo, key):
                                for qs in range(4):
                                    S.op('dve', lambda e, qs=qs: e.reciprocal(out=nst[:, 4 + qs:5 + qs], in_=accs[qs][:, 128:129]), [acck[qs]], ['nst2'])
                                    ts(yo[:, qs, :], accs[qs][:, 0:128], nst[:, 4 + qs:5 + qs], None, ALU.mult, None, [acck[qs], 'nst2'], [key])
                            store_y(qb, 1536 + h * 128, fill)
                        attn_core(1, [(QT[:, hh, :], 128)], [(KT[:, hh, :], 128)], [f'mq{hh}'], [f'mk{hh}', 'selB'],
                                  lambda kt, hh=hh: V[:, kt, hh, 0:129], ['mbv'], 129, score, False, fin)

            def mixer_mla():
                A.off = mixer_base
                cqnT = A.bf16(4 * SEQ).rearrange("p (k t) -> p k t", t=SEQ)
                ckvnT = A.bf16(2 * SEQ).rearrange("p (k t) -> p k t", t=SEQ)
                kr = A.f32(16 * 64).rearrange("p (t d) -> p t d", d=64)
                sskpe = A.f32(16)
                QT = A.bf16(2 * SEQ).rearrange("p (k t) -> p k t", t=SEQ)
                KT = A.bf16(2 * SEQ).rearrange("p (k t) -> p k t", t=SEQ)
                V = A.bf16(16 * 130).rearrange("p (t c) -> p t c", c=130)
                wuq = A.bf16(4 * 192).rearrange("p (k n) -> p k n", n=192)
                wukv = A.bf16(2 * 256).rearrange("p (k n) -> p k n", n=256)
                cq = A.f32(512)
                rt = A.f32(192)
                ra = A.f32(64)
                rb = A.f32(64)
                kn = A.f32(192)
                nst = A.f32(16)
                t_e = A.f32(512)
                gq = rowp[:, 0:192]
                gk = rowp[:, 192:384]
                qng = colp[:, l * NCOL + 96:l * NCOL + 100]
                kvng = colp[:, l * NCOL + 100:l * NCOL + 102]

                def cb_cq(half):
                    def cb(t, ps, pkey):
                        act(junk[:, 0:256], ps, AF.Square, [pkey], ['ssq'], accum_out=nst[:, half:half + 1])
                        cp(cq[:, half * 256:(half + 1) * 256], ps, [pkey, 'ssq'], ['cq'])
                        if half == 1:
                            tt(nst[:, 2:3], nst[:, 0:1], nst[:, 1:2], ALU.add, ['ssq'], ['ssq'])
                            rstd_from_ss(nst[:, 3:4], nst[:, 2:3], 512, ['ssq'])
                            act(cq, cq, AF.Identity, ['cq', 'ssq'], ['cq'], scale=nst[:, 3:4])
                            p = PS[2 + (t % 2)]
                            for kc in range(4):
                                tr(p[:, kc * 128:(kc + 1) * 128], cq[:, kc * 128:(kc + 1) * 128], ['cq'], [pk[2 + (t % 2)]])
                            for kc in range(4):
                                act(cqnT[:, kc, t * 128:(t + 1) * 128], p[:, kc * 128:(kc + 1) * 128], AF.Identity, [pk[2 + (t % 2)], 'const'],
                                    ['cqnT'], scale=qng[:, kc:kc + 1])
                    return cb
                w0, wk0 = load_w(wbufs, 'win', win[:, 0:256], 16, 256)
                w1, wk1 = load_w(wbufs, 'win', win[:, 256:512], 16, 256)
                def grp_cq(t):
                    for half, (w, wk) in enumerate([(w0, wk0), (w1, wk1)]):
                        pi = 4 + 2 * (t % 2) + half
                        for kc in range(16):
                            mm(PS[pi][:, 0:256], hT[:, kc, t * 128:(t + 1) * 128], w[:, kc, :], kc == 0, kc == 15, [wk, f'hT{t}'], [pk[pi]])
                grp_cq(0)
                for t in range(16):
                    if t + 1 < 16:
                        grp_cq(t + 1)
                    for half in range(2):
                        pi = 4 + 2 * (t % 2) + half
                        cb_cq(half)(t, PS[pi][:, 0:256], pk[pi])

                import os
                if int(os.environ.get('MLA_STAGE', '9')) <= 0:
                    return

                def cb_ckv(t, ps, pkey):
                    act(junk[:, 0:256], ps, AF.Square, [pkey], ['sskv'], accum_out=nst[:, 4:5])
                    rstd_from_ss(nst[:, 5:6], nst[:, 4:5], 256, ['sskv'])
                    act(cq[:, 0:256], ps, AF.Identity, [pkey, 'sskv'], ['cq'], scale=nst[:, 5:6])
                    p = PS[2 + (t % 2)]
                    for kc in range(2):
                        tr(p[:, kc * 128:(kc + 1) * 128], cq[:, kc * 128:(kc + 1) * 128], ['cq'], [pk[2 + (t % 2)]])
                    for kc in range(2):
                        act(ckvnT[:, kc, t * 128:(t + 1) * 128], p[:, kc * 128:(kc + 1) * 128], AF.Identity, [pk[2 + (t % 2)], 'const'],
                            ['ckvnT'], scale=kvng[:, kc:kc + 1])
                proj_tm(512, 256, cb_ckv)

                def cb_kpe(t, ps, pkey):
                    act(junk[:, 0:64], ps, AF.Square, [pkey], ['sskpe'], accum_out=sskpe[:, t:t + 1])
                    tt(rt[:, 0:64], ps, gk[:, 128:192], ALU.mult, [pkey, 'mconst'], ['rt'])
                    rope_tm(kr[:, t, :], rt[:, 0:64], ['rt', 'kr'], cs_pe, t, 32, ra, rb)
                proj_tm(768, 64, cb_kpe)

                scale = 192 ** -0.5
                kbias = A.f32(16)
                ts(kbias, sskpe, 1.0 / 192, EPS, ALU.mult, ALU.add, ['sskpe'], ['kbias'])
                import os
                MST = int(os.environ.get('MLA_STAGE', '9'))
                if MST <= 1:
                    return
                wuq_d = dr['mla_w_uq'][l]
                wukv_d = dr['mla_w_ukv'][l]
                for h in range(4):
                    S.dma('pool', wuq, wuq_d[:, h * 192:(h + 1) * 192].rearrange("(kc p) n -> p kc n", p=128), (), ['wuq'])
                    S.dma('pool', wukv, wukv_d[:, h * 256:(h + 1) * 256].rearrange("(kc p) n -> p kc n", p=128), (), ['wukv'])
                    if h == 0:
                        mset(V[:, :, 128:129], 1.0, ['mlav'])
                    def up(t):
                        bq = 4 + 2 * (t % 2)
                        for kc in range(4):
                            mm(PS[bq][:, 0:192], cqnT[:, kc, t * 128:(t + 1) * 128], wuq[:, kc, :], kc == 0, kc == 3, ['cqnT', 'wuq'], [pk[bq]])
                        for kc in range(2):
                            mm(PS[bq + 1][:, 0:256], ckvnT[:, kc, t * 128:(t + 1) * 128], wukv[:, kc, :], kc == 0, kc == 1, ['ckvnT', 'wukv'], [pk[bq + 1]])
                    up(0)
                    for t in range(16):
                        if t + 1 < 16:
                            up(t + 1)
                        bq = 4 + 2 * (t % 2)
                        pq = PS[bq]
                        pkq = pk[bq]
                        pkv = PS[bq + 1]
                        pkvk = pk[bq + 1]
                        act(junk[:, 0:192], pq[:, 0:192], AF.Square, [pkq], ['nq'], accum_out=nst[:, 6:7])
                        rstd_from_ss(nst[:, 7:8], nst[:, 6:7], 192, ['nq'])
                        act(junk[:, 0:128], pkv[:, 0:128], AF.Square, [pkvk], ['nk'], accum_out=nst[:, 8:9])
                        act(V[:, t, 0:128], pkv[:, 128:256], AF.Identity, [pkvk], ['mlav'])
                        act(nst[:, 9:10], nst[:, 8:9], AF.Ln, ['nk', 'kbias'], ['nk'], scale=1.0 / 192, bias=kbias[:, t:t + 1])
                        act(nst[:, 9:10], nst[:, 9:10], AF.Exp, ['nk'], ['nk'], scale=-0.5)
                        stt(rt, pq[:, 0:192], nst[:, 7:8], gq, ALU.mult, ALU.mult, [pkq, 'nq', 'mconst'], ['rt'])
                        rope_tm(rt[:, 128:192], rt[:, 128:192], ['rt'], cs_pe, t, 32, ra, rb)
                        p = PS[2 + (t % 2)]
                        tr(p[:, 0:128], rt[:, 0:128], ['rt'], [pk[2 + (t % 2)]])
                        tr(p[0:64, 128:256], rt[:, 128:192], ['rt'], [pk[2 + (t % 2)]])
                        stt(kn[:, 0:128], pkv[:, 0:128], nst[:, 9:10], gk[:, 0:128], ALU.mult, ALU.mult, [pkvk, 'nk', 'mconst'], ['kn'])
                        ts(kn[:, 128:192], kr[:, t, :], nst[:, 9:10], None, ALU.mult, None, ['kr', 'nk'], ['kn'])
                        p2 = PS[2 + ((t + 1) % 2)]
                        tr(p2[:, 256:384], kn[:, 0:128], ['kn'], [pk[2 + ((t + 1) % 2)]])
                        tr(p2[0:64, 384:512], kn[:, 128:192], ['kn'], [pk[2 + ((t + 1) % 2)]])
                        act(QT[:, 0, t * 128:(t + 1) * 128], p[:, 0:128], AF.Identity, [pk[2 + (t % 2)]], ['mlaq'])
                        act(QT[0:64, 1, t * 128:(t + 1) * 128], p[0:64, 128:256], AF.Identity, [pk[2 + (t % 2)]], ['mlaq'])
                        act(KT[:, 0, t * 128:(t + 1) * 128], p2[:, 256:384], AF.Identity, [pk[2 + ((t + 1) % 2)]], ['mlak'])
                        act(KT[0:64, 1, t * 128:(t + 1) * 128], p2[0:64, 384:512], AF.Identity, [pk[2 + ((t + 1) % 2)]], ['mlak'])

                    def score(qb, kt, j, c0, N, ps_s, psk, PT, ptk):
                        if j >= 0:
                            act(t_e[:, 0:N], ps_s, AF.Exp, [psk], ['t_e'], scale=scale)
                            tt(PT[:, c0:512], t_e[:, 0:N], cmask[:, 0:N], ALU.mult, ['t_e', 'mconst'], [ptk])
                        else:
                            act(PT[:, c0:512], ps_s, AF.Exp, [psk], [ptk], scale=scale)

                    def fin(qb, accs, acck, h=h):
                        def fill(yo, key):
                            for qs in range(4):
                                S.op('dve', lambda e, qs=qs: e.reciprocal(out=nst[:, 10 + qs:11 + qs], in_=accs[qs][:, 128:129]), [acck[qs]], ['nst2'])
                                ts(yo[:, qs, :], accs[qs][:, 0:128], nst[:, 10 + qs:11 + qs], None, ALU.mult, None, [acck[qs], 'nst2'], [key])
                        store_y(qb, h * 128, fill)
                    if MST <= 2:
                        continue
                    attn_core(2, [(QT[:, 0, :], 128), (QT[:, 1, :], 64)], [(KT[:, 0, :], 128), (KT[:, 1, :], 64)], ['mlaq'], ['mlak'],
                              lambda kt: V[:, kt, 0:129], ['mlav'], 129, score, False, fin)

            which = MIXERS
            if 'mla' in which:
                mixer_mla()
                S.barrier()
            if 'ret' in which:
                mixer_ret()
                S.barrier()
            if 'sb' in which:
                mixer_sb()
                S.barrier()
            if 'moba' in which:
                mixer_moba()
                S.barrier()

            A.off = mixer_base
            gateB = A.f32(D)
            xs = [A.f32(256) for _ in range(3)]
            ys = [A.f32(256) for _ in range(3)]
            S.dma('sp', gateB, gate_d[:, (l * 2) * D:(l * 2 + 1) * D], (), ['gateB'])
            gng = colp[:, l * NCOL + 102:l * NCOL + 118]
            norm_T(ycat_d, hT, 'hT', 0, 16, 4, gng, None, xt_bufs, junk, stat)
            wout = dr['w_out'][l]
            ctr = 0
            for cb_ in range(8):
                w, wk = load_w(wbufs, 'win', wout[:, cb_ * 256:(cb_ + 1) * 256], 16, 256)
                for t in range(16):
                    pi = 6 + (t % 2)
                    for kc in range(16):
                        mm(PS[pi][:, 0:256], hT[:, kc, t * 128:(t + 1) * 128], w[:, kc, :], kc == 0, kc == 15, [wk, f'hT{t}'], [pk[pi]])
                    i = ctr % 3
                    ctr += 1
                    S.dma('sp', xs[i], src_d[t * 128:(t + 1) * 128, cb_ * 256:(cb_ + 1) * 256], (), [f'xs{i}'])
                    tt(ys[i], PS[pi][:, 0:256], gateB[:, cb_ * 256:(cb_ + 1) * 256], ALU.mult, [pk[pi], 'gateB'], [f'ys{i}'])
                    tt(ys[i], ys[i], xs[i], ALU.add, [f'ys{i}', f'xs{i}'], [f'ys{i}'])
                    S.dma('sp', x1_d[t * 128:(t + 1) * 128, cb_ * 256:(cb_ + 1) * 256], ys[i], [f'ys{i}'], [f'x1d{t}_{cb_}'])
            S.barrier()
            A.pop()

        def phase_ffn(l, src_d, dst_d):
            A.push()
            moe = (l == 1)
            nexp = 8 if moe else 1
            h2T = A.bf16(16 * 1024).rearrange("p (k t) -> p k t", t=1024)
            gT = A.bf16(11 * 1024).rearrange("p (f t) -> p f t", t=1024)
            yacc = A.f32(8 * D).rearrange("p (t d) -> p t d", d=D)
            w1b = [A.bf16(16 * 128).rearrange("p (k n) -> p k n", n=128) for _ in range(2)]
            w3b = [A.bf16(16 * 128).rearrange("p (k n) -> p k n", n=128) for _ in range(2)]
            w2b = [A.bf16(11 * 512).rearrange("p (f n) -> p f n", n=512) for _ in range(2)]
            xt_bufs = [A.f32(D) for _ in range(2)]
            junk = A.bf16(D)
            stat = A.f32(16)
            gateB = A.f32(D)
            su = A.f32(512)
            S.dma('sp', gateB, gate_d[:, (l * 2 + 1) * D:(l * 2 + 2) * D], (), ['gateB'])
            if moe:
                sel8 = A.f32(1024).rearrange("p (e m) -> p e m", m=128)
                rw = A.f32(16 * 8).rearrange("p (k e) -> p k e", e=8)
                hf = A.f32(128)
                lg = A.f32(8)
                top8 = A.f32(8)
                gsel = A.f32(8)
                G = A.f32(8 * 8).rearrange("p (t e) -> p t e", e=8)
                GT = A.f32(1024)
                egate = A.f32(1024)
                mst = A.f32(8)
                S.dma('sp', sel8[0:8].rearrange("p e m -> p (e m)"), dr['sel8'], (), ['sel8'])
                S.dma('sp', rw, dr['router_w'][0].rearrange("(kc p) e -> p kc e", p=128), (), ['rw'])
            af, bf = mod_cols(l, 'f')
            for th in range(2):
                hook = None
                if moe:
                    def hook(t, dc, ps, pkey, th=th):
                        tl = t - th * 8
                        act(hf, ps, AF.Identity, [pkey, 'modc1'], ['hf'], scale=af[:, dc:dc + 1], bias=bf[:, dc:dc + 1])
                        mm(PS[2][:, tl * 8:tl * 8 + 8], hf, rw[:, dc, :], dc == 0, dc == 15, ['hf', 'rw'], [pk[2]], sig=True)
                norm_T(src_d, h2T, f'h2T{th}_', th * 8, th * 8 + 8, 1, af, bf, xt_bufs, junk, stat, hook)
                hk = [f'h2T{th}_{t}' for t in range(th * 8, th * 8 + 8)]
                if moe:
                    for tl in range(8):
                        cp(lg, PS[2][:, tl * 8:tl * 8 + 8], [pk[2]], ['lg'])
                        S.op('dve', lambda e: e.max(out=top8, in_=lg), ['lg'], ['top8'])
                        tt(mst[:, 0:1], top8[:, 1:2], top8[:, 0:1], ALU.subtract, ['top8'], ['mst'])
                        act(mst[:, 1:2], mst[:, 0:1], AF.Exp, ['mst'], ['mst'])
                        ts(mst[:, 1:2], mst[:, 1:2], 1.0, None, ALU.add, None, ['mst'], ['mst'])
                        S.op('dve', lambda e: e.reciprocal(out=mst[:, 2:3], in_=mst[:, 1:2]), ['mst'], ['mst'])
                        ts(gsel, lg, top8[:, 1:2], None, ALU.is_ge, None, ['lg', 'top8'], ['gsel'])
                        ts(mst[:, 3:4], top8[:, 0:1], -1.0, None, ALU.mult, None, ['top8', 'mst'], ['mst'])
                        act(lg, lg, AF.Exp, ['lg', 'mst'], ['lg'], bias=mst[:, 3:4])
                        stt(G[:, tl, :], lg, mst[:, 2:3], gsel, ALU.mult, ALU.mult, ['lg', 'mst', 'gsel'], ['G'])
                        if tl < 4:
                            tr(PS[3][0:8, tl * 128:(tl + 1) * 128], G[:, tl, :], ['G'], [pk[3]])
                        else:
                            tr(PS[1][0:8, (tl - 4) * 128:(tl - 3) * 128], G[:, tl, :], ['G'], [pk[1]])
                    cp(GT[0:8, 0:512], PS[3][0:8, :], [pk[3]], ['GT'])
                    cp(GT[0:8, 512:1024], PS[1][0:8, :], [pk[1]], ['GT'])
                for e in range(nexp):
                    if moe:
                        w1d, w3d, w2d = dr['moe_w1'][0, e], dr['moe_w3'][0, e], dr['moe_w2'][0, e]
                        for tb in range(2):
                            mm(PS[3], sel8[0:8, e, :], GT[0:8, tb * 512:(tb + 1) * 512], True, True, ['sel8', 'GT'], [pk[3]])
                            act(egate[:, tb * 512:(tb + 1) * 512], PS[3], AF.Identity, [pk[3]], ['egate'])
                    else:
                        w1d, w3d, w2d = dr['ffn_w1'][0], dr['ffn_w3'][0], dr['ffn_w2'][0]
                    for fq in range(4):
                        for fl in range(11):
                            fc = fq * 11 + fl
                            w1, k1 = load_w(w1b, 'w1', w1d[:, fc * 128:(fc + 1) * 128], 16, 128)
                            wctr[0] -= 1
                            w3, k3 = load_w(w3b, 'w3', w3d[:, fc * 128:(fc + 1) * 128], 16, 128)
                            for tb in range(2):
                                pu = PS[0 + tb]
                                pv = PS[4 + tb]
                                for kc in range(16):
                                    mm(pu, w1[:, kc, :], h2T[:, kc, tb * 512:(tb + 1) * 512], kc == 0, kc == 15, [k1] + hk[tb * 4:tb * 4 + 4], [pk[tb]])
                                for kc in range(16):
                                    mm(pv, w3[:, kc, :], h2T[:, kc, tb * 512:(tb + 1) * 512], kc == 0, kc == 15, [k3] + hk[tb * 4:tb * 4 + 4], [pk[4 + tb]])
                                act(su, pu, AF.Silu, [pk[tb]], ['su'])
                                if moe:
                                    tt(su, su, egate[:, tb * 512:(tb + 1) * 512], ALU.mult, ['su', 'egate'], ['su'])
                                tt(gT[:, fl, tb * 512:(tb + 1) * 512], su, pv, ALU.mult, ['su', pk[4 + tb]], [f'gT{tb}'])
                        for db in range(4):
                            i2 = (fq * 4 + db) % 2
                            w2 = w2b[i2]
                            k2 = f'w2_{i2}'
                            S.dma('pool', w2, w2d[fq * 1408:(fq + 1) * 1408, db * 512:(db + 1) * 512].rearrange("(f p) n -> p f n", p=128), (), [k2])
                            for tl in range(8):
                                pi = 6 + (tl % 2)
                                for fl in range(11):
                                    mm(PS[pi], gT[:, fl, tl * 128:(tl + 1) * 128], w2[:, fl, :], fl == 0, fl == 10, [k2, f'gT{tl // 4}'], [pk[pi]])
                                ya = yacc[:, tl, db * 512:(db + 1) * 512]
                                if e == 0 and fq == 0:
                                    cp(ya, PS[pi], [pk[pi]], [f'yacc{tl}'])
                                else:
                                    tt(ya, ya, PS[pi], ALU.add, [pk[pi], f'yacc{tl}'], [f'yacc{tl}'])
                for tl in range(8):
                    t = th * 8 + tl
                    xt = xt_bufs[t % 2]
                    xk = f'xt{t % 2}'
                    S.dma('sp', xt, src_d[t * 128:(t + 1) * 128, :], (), [xk])
                    tt(yacc[:, tl, :], yacc[:, tl, :], gateB, ALU.mult, [f'yacc{tl}', 'gateB'], [f'yacc{tl}'])
                    tt(xt, xt, yacc[:, tl, :], ALU.add, [xk, f'yacc{tl}'], [xk])
                    S.dma('sp', dst_d[t * 128:(t + 1) * 128, :], xt, [xk], [f'dst{t}'])
            S.barrier()
            A.pop()

        phase_ada()
        stages = [('mix', 0, dr['x']), ('ffn', 0, x1_d, x2_d), ('mix', 1, x2_d), ('ffn', 1, x1_d, out_d)]
        n = len(stages) if stop_after is None else stop_after
        for st in stages[:n]:
            if st[0] == 'mix':
                phase_mixer(st[1], st[2])
            else:
                phase_ffn(st[1], st[2], st[3])
        S.barrier()
        S.emit_all()
    return nc


MIXERS = ('mla', 'ret', 'sb', 'moba')


def host_inputs(inputs):
    f = lambda a: np.ascontiguousarray(np.asarray(a, dtype=np.float32))
    consts = host_consts()
    colp = np.zeros((128, 2 * NCOL), np.float32)
    rowp = np.zeros((128, 2 * NROW), np.float32)
    adabr = np.zeros((128, 4 * D), np.float32)

    def col(v):
        return np.asarray(v, np.float32).reshape(-1, 128).T

    for l in range(2):
        ab = np.asarray(inputs['ada_b'][l], np.float32)
        o = l * NCOL
        for i, gi in enumerate([0, 1, 3, 4]):
            colp[:, o + i * 16:o + (i + 1) * 16] = col(ab[gi * D:(gi + 1) * D])
        colp[:, o + 64:o + 80] = col(inputs['norm_mix_g'][l])
        colp[:, o + 80:o + 96] = col(inputs['norm_ffn_g'][l])
        colp[:, o + 96:o + 100] = col(inputs['mla_q_norm_g'][l])
        colp[:, o + 100:o + 102] = col(inputs['mla_kv_norm_g'][l])
        colp[:, o + 102:o + 118] = col(np.asarray(inputs['group_norm_g'][l]).reshape(-1))
        r = l * NROW
        rowp[:, r:r + 192] = np.asarray(inputs['mla_q_head_g'][l], np.float32)[None, :]
        rowp[:, r + 192:r + 384] = np.asarray(inputs['mla_k_head_g'][l], np.float32)[None, :]
        rowp[:, r + 384:r + 512] = np.asarray(inputs['moba_q_head_g'][l], np.float32)[None, :]
        rowp[:, r + 512:r + 640] = np.asarray(inputs['moba_k_head_g'][l], np.float32)[None, :]
        adabr[:, (l * 2) * D:(l * 2 + 1) * D] = ab[2 * D:3 * D][None, :]
        adabr[:, (l * 2 + 1) * D:(l * 2 + 2) * D] = ab[5 * D:6 * D][None, :]
    shared = {'colp': colp, 'rowp': rowp, 'adabr': adabr}
    shared.update(consts)
    for k, _ in WEIGHTS:
        shared[k] = f(inputs[k])
    pos = np.ascontiguousarray(np.asarray(inputs['positions'], np.int32).reshape(16, 128).T)
    x = np.asarray(inputs['x'], np.float32)
    c = np.asarray(inputs['c'], np.float32)
    maps = []
    for b in range(8):
        m = dict(shared)
        m['x'] = np.ascontiguousarray(x[b])
        m['c_col'] = np.ascontiguousarray(c[b].reshape(16, 128).T)
        m['pos'] = pos
        maps.append(m)
    return maps


_NC_CACHE = {}


def kernel(**inputs):
    maps = host_inputs(inputs)
    if 'nc' not in _NC_CACHE:
        _NC_CACHE['nc'] = build()
    nc = _NC_CACHE['nc']
    res = run_bass_kernel_spmd(nc, maps, core_ids=list(range(8)))
    return np.stack([np.asarray(r['out'], dtype=np.float32) for r in res.results], axis=0)
```

```python
import math
from contextlib import ExitStack
import numpy as np
import concourse.bass as bass
import concourse.mybir as mybir
from concourse.bass_utils import run_bass_kernel_spmd

F32 = mybir.dt.float32
BF16 = mybir.dt.bfloat16
I32 = mybir.dt.int32
AF = mybir.ActivationFunctionType
ALU = mybir.AluOpType
AX = mybir.AxisListType

D = 2048
SEQ = 2048
NT = 16
FF = 5632
NFC = 44
INW = 5952
EPS = 1e-6
EPOCH = 6000
ARENA = 53000


class Sch:
    ENG = ['pe', 'act', 'dve', 'pool', 'sp']

    def __init__(self, nc, es):
        self.nc = nc
        self.es = es
        self.q = {e: [] for e in self.ENG}
        self.semh = {}
        self.cnt = {e: 0 for e in ['pe', 'act', 'dve', 'pool']}
        self.ep = {e: 0 for e in ['pe', 'act', 'dve', 'pool']}
        self.waited = {e: {} for e in self.ENG}
        self.lastw = {}
        self.readers = {}
        self.nds = 24
        self.dcnt = [0] * self.nds
        self.dnext = 0

    def sem(self, name):
        if name not in self.semh:
            self.semh[name] = self.es.enter_context(self.nc.semaphore(name))
        return self.semh[name]

    def _need(self, reads, writes, eng=None):
        need = {}

        def add(st):
            if st is not None and need.get(st[0], 0) < st[1]:
                need[st[0]] = st[1]
        for k in reads:
            add(self.lastw.get(k))
            if isinstance(k, str) and k.startswith('ps') and eng is not None:
                for s, v in self.readers.get(k, {}).items():
                    if not s.startswith(eng):
                        add((s, v))
        for k in writes:
            add(self.lastw.get(k))
            for s, v in self.readers.get(k, {}).items():
                add((s, v))
        return need

    def _waits(self, eng, need):
        for s, v in need.items():
            if eng == 'pe' and s.startswith('pe'):
                continue
            if self.waited[eng].get(s, 0) >= v:
                continue
            self.waited[eng][s] = v
            self.sem(s)
            self.q[eng].append(('w', s, v))

    def _mark(self, st, reads, writes):
        for k in writes:
            self.lastw[k] = st
            self.readers[k] = {}
        for k in reads:
            d = self.readers.setdefault(k, {})
            if d.get(st[0], 0) < st[1]:
                d[st[0]] = st[1]

    def op(self, eng, emit, reads=(), writes=(), signal=True):
        self._waits(eng, self._need(reads, writes, eng))
        if signal:
            if self.cnt[eng] >= EPOCH:
                self.ep[eng] += 1
                self.cnt[eng] = 0
            name = f"{eng}{self.ep[eng]}"
            self.sem(name)
            self.cnt[eng] += 1
            st = (name, self.cnt[eng])
            self.q[eng].append(('i', emit, name))
        else:
            if self.cnt[eng] >= EPOCH:
                st = (f"{eng}{self.ep[eng] + 1}", 1)
            else:
                st = (f"{eng}{self.ep[eng]}", self.cnt[eng] + 1)
            self.sem(st[0])
            self.q[eng].append(('n', emit))
        self._mark(st, reads, writes)

    def dma(self, eng, out, in_, reads=(), writes=()):
        i = self.dnext
        self.dnext = (i + 1) % self.nds
        name = f"dq{i}"
        self.sem(name)
        need = self._need(reads, writes)
        if self.dcnt[i] > 0:
            need[name] = 16 * self.dcnt[i]
        self._waits(eng, need)
        self.dcnt[i] += 1
        st = (name, 16 * self.dcnt[i])
        self.q[eng].append(('d', out, in_, name))
        self._mark(st, reads, writes)

    def barrier(self):
        cur = {}
        for e in self.cnt:
            if self.cnt[e] > 0:
                cur[f"{e}{self.ep[e]}"] = self.cnt[e]
        for i in range(self.nds):
            if self.dcnt[i] > 0:
                cur[f"dq{i}"] = 16 * self.dcnt[i]
        for e in self.ENG:
            self._waits(e, dict(cur))
        self.lastw.clear()
        self.readers.clear()

    def emit_all(self):
        nc = self.nc
        block = self.es.enter_context(nc.Block())
        semh = self.semh

        def run(lst):
            def f(e):
                for it in lst:
                    if it[0] == 'w':
                        e.wait_ge(semh[it[1]], it[2])
                    elif it[0] == 'i':
                        it[1](e).then_inc(semh[it[2]], 1)
                    elif it[0] == 'n':
                        it[1](e)
                    else:
                        e.dma_start(out=it[1], in_=it[2]).then_inc(semh[it[3]], 16)
            return f
        block.tensor(run(self.q['pe']))
        block.scalar(run(self.q['act']))
        block.vector(run(self.q['dve']))
        block.gpsimd(run(self.q['pool']))
        block.sync(run(self.q['sp']))


class Arena:
    def __init__(self, ap):
        self.ap = ap
        self.off = 0
        self.marks = []

    def push(self):
        self.marks.append(self.off)

    def pop(self):
        self.off = self.marks.pop()

    def f32(self, n):
        o = self.off
        self.off += n
        assert self.off <= ARENA, f"arena overflow {self.off}"
        return self.ap[:, o:o + n]

    def bf16(self, n):
        nf = (n + 1) // 2
        return self.f32(nf).bitcast(BF16)[:, 0:n]


def host_consts():
    c = {}
    c['ident'] = np.eye(128, dtype=np.float32)
    ki = np.arange(128)[:, None]
    xx = np.arange(512)[None, :]
    c['cmask'] = (xx >= ki).astype(np.float32)
    c['smask'] = (xx > ki).astype(np.float32)
    jj = np.arange(128)[:, None]
    kk = np.arange(128)[None, :]
    c['umat'] = (jj > kk).astype(np.float32)
    c['ones'] = np.ones((128, 128), np.float32)
    inv_pe = 10000.0 ** (-np.arange(0, 64, 2, dtype=np.float32) / 64)
    inv_full = 10000.0 ** (-np.arange(0, 128, 2, dtype=np.float32) / 128)
    c['invf'] = np.tile(np.concatenate([inv_pe, inv_full])[None, :].astype(np.float32), (128, 1))
    rett = np.zeros((128, 4, 2, 512), np.float32)
    for h in range(4):
        lg = math.log(1.0 - 2.0 ** (-5.0 - h))
        e = (xx - ki).astype(np.float64)
        rett[:, h, 0, :] = np.exp(e * lg)
        rett[:, h, 1, :] = np.where(e >= 0, np.exp(np.maximum(e, 0) * lg), 0.0)
    c['rett'] = rett.reshape(128, 4096)
    sel8 = np.zeros((8, 8, 128), np.float32)
    for e in range(8):
        sel8[e, e, :] = 1.0
    c['sel8'] = sel8.reshape(8, 1024)
    mc = np.zeros((128, 3, 16, 8), np.float32)
    for t in range(16):
        cur = t // 2
        for n in range(8):
            mc[:, 0, t, n] = 1.0 if n < cur else 0.0
            mc[:, 1, t, n] = 0.0 if n < cur else -1e30
            mc[:, 2, t, n] = 1.0 if n == cur else 0.0
    c['mobac'] = mc.reshape(128, 384)
    return c


CONST_SHAPES = {'ident': [128, 128], 'cmask': [128, 512], 'smask': [128, 512], 'umat': [128, 128],
                'ones': [128, 128], 'invf': [128, 96], 'rett': [128, 4096], 'sel8': [8, 1024], 'mobac': [128, 384]}

NCOL = 64 + 16 + 16 + 4 + 2 + 16
NROW = 640

WEIGHTS = [("ada_w", [2, D, 6 * D]), ("w_in", [2, D, INW]), ("mla_w_uq", [2, 512, 768]), ("mla_w_ukv", [2, 256, 1024]),
           ("w_out", [2, D, D]), ("ffn_w1", [1, D, FF]), ("ffn_w3", [1, D, FF]), ("ffn_w2", [1, FF, D]),
           ("router_w", [1, D, 8]), ("moe_w1", [1, 8, D, FF]), ("moe_w3", [1, 8, D, FF]), ("moe_w2", [1, 8, FF, D])]

GAMMA = [1.0 - 2.0 ** (-5.0 - h) for h in range(4)]


def build(stop_after=None, debug=False):
    nc = bass.Bass("TRN2", target_bir_lowering=False)
    dr = {}

    def din(name, shape, dt=F32):
        dr[name] = nc.dram_tensor(name, shape, dt, kind="ExternalInput").ap()
        return dr[name]

    din("x", [SEQ, D])
    din("c_col", [128, 16])
    din("pos", [128, 16], I32)
    din("colp", [128, 2 * NCOL])
    din("rowp", [128, 2 * NROW])
    din("adabr", [128, 2 * 2 * D])
    for k, shp in CONST_SHAPES.items():
        din(k, shp)
    nst_ = 4 if stop_after is None else stop_after
    for k, shp in WEIGHTS:
        if k.startswith('ffn') and nst_ < 2:
            continue
        if (k.startswith('moe') or k.startswith('router')) and nst_ < 4:
            continue
        din(k, shp)
    dbg_outs = {}

    def dbg_dump(S, name, ap, shape, dt, keys):
        if not debug:
            return
        t = nc.dram_tensor(name, shape, dt, kind="ExternalOutput").ap()
        S.dma('sp', t, ap, keys, [f'dbg_{name}'])
    out_d = nc.dram_tensor("out", [SEQ, D], F32, kind="ExternalOutput").ap()
    skind = "ExternalOutput" if debug else "Internal"
    ycat_d = nc.dram_tensor("ycat_raw", [SEQ, D], F32, kind=skind).ap()
    x1_d = nc.dram_tensor("x1s", [SEQ, D], F32, kind=skind).ap()
    x2_d = nc.dram_tensor("x2s", [SEQ, D], F32, kind=skind).ap()
    gate_d = nc.dram_tensor("gaterow", [128, 4 * D], F32, kind=skind).ap()

    es = ExitStack()
    with es:
        arena_t = es.enter_context(nc.sbuf_tensor("arena", [128, ARENA], F32))
        A = Arena(arena_t)
        PS = [es.enter_context(nc.psum_tensor(f"ps{i}", [128, 512], F32))[:, :] for i in range(8)]
        S = Sch(nc, es)
        pk = [f"ps{i}" for i in range(8)]

        def act(out, in_, func, reads, writes, **kw):
            S.op('act', lambda e: e.activation(out=out, in_=in_, func=func, **kw), reads, writes)

        def ts(out, in0, s1, s2, op0, op1, reads, writes, eng='dve'):
            if op1 is None:
                S.op(eng, lambda e: e.tensor_scalar(out=out, in0=in0, scalar1=s1, scalar2=None, op0=op0), reads, writes)
            else:
                S.op(eng, lambda e: e.tensor_scalar(out=out, in0=in0, scalar1=s1, scalar2=s2, op0=op0, op1=op1), reads, writes)

        def tt(out, in0, in1, op, reads, writes, eng='dve'):
            S.op(eng, lambda e: e.tensor_tensor(out=out, in0=in0, in1=in1, op=op), reads, writes)

        def stt(out, in0, sc, in1, op0, op1, reads, writes, eng='dve'):
            S.op(eng, lambda e: e.scalar_tensor_tensor(out=out, in0=in0, scalar=sc, in1=in1, op0=op0, op1=op1), reads, writes)

        def cp(out, in_, reads, writes, eng='dve'):
            S.op(eng, lambda e: e.tensor_copy(out=out, in_=in_), reads, writes)

        def mset(ap, val, writes, eng='dve'):
            S.op(eng, lambda e: e.memset(ap, val), (), writes)

        def mm(out, lhsT, rhs, start, stop, reads, writes, sig=None):
            S.op('pe', lambda e: e.matmul(out, lhsT=lhsT, rhs=rhs, start=start, stop=stop), reads, writes,
                 signal=(stop if sig is None else sig))

        def tr(out, in_, reads, writes):
            S.op('pe', lambda e: e.transpose(out, in_, ident), list(reads) + ['const'], writes)

        def rstd_from_ss(rs, ss, n, keys):
            act(rs, ss, AF.Ln, keys, keys, scale=1.0 / n, bias=EPS)
            act(rs, rs, AF.Exp, keys, keys, scale=-0.5)

        ident = A.f32(128)
        colp = A.f32(2 * NCOL)
        modc = A.f32(2 * 64)
        S.dma('sp', ident, dr['ident'], (), ['const'])
        S.dma('sp', colp, dr['colp'], (), ['const'])

        def phase_ada():
            A.push()
            ones = A.f32(128)
            ccol = A.f32(16)
            cond = A.f32(16)
            condrep = A.f32(16 * 128).rearrange("p (k m) -> p k m", m=128)
            wb = [A.f32(16 * 512).rearrange("p (k n) -> p k n", n=512) for _ in range(2)]
            brow = A.f32(512)
            orow = [A.f32(512) for _ in range(2)]
            mtmp = A.f32(64)
            S.dma('sp', ones, dr['ones'], (), ['ones'])
            S.dma('sp', ccol, dr['c_col'], (), ['ccol'])
            act(cond, ccol, AF.Silu, ['ccol'], ['cond'])
            for kc in range(16):
                ts(condrep[:, kc, :], ones, cond[:, kc:kc + 1], None, ALU.mult, None, ['ones', 'cond'], [f'crep{kc}'])
            wi = 0
            for l in range(2):
                awv = dr['ada_w'][l].rearrange("(kc p) n -> p kc n", p=128)
                for gi_i, gi in enumerate([0, 1, 3, 4]):
                    for jb in range(4):
                        w = wb[wi % 2]
                        wk = f'adaw{wi % 2}'
                        wi += 1
                        S.dma('sp', w, awv[:, :, gi * D + jb * 512: gi * D + (jb + 1) * 512], (), [wk])
                        for kc in range(16):
                            mm(PS[1], condrep[:, kc, :], w[:, kc, :], kc == 0, kc == 15, [wk, f'crep{kc}'], [pk[1]])
                        o = orow[jb % 2]
                        ok_ = f'orow{jb % 2}'
                        cp(o, PS[1], [pk[1]], [ok_])
                        for j in range(4):
                            tr(PS[2][:, j * 128:(j + 1) * 128], o[:, j * 128:(j + 1) * 128], [ok_], [pk[2]])
                        col = gi_i * 16 + jb * 4
                        cp(mtmp[:, col:col + 4], PS[2].rearrange("p (j m) -> p j m", m=128)[:, :, 0], [pk[2]], ['mtmp'])
                mraw = modc[:, l * 64:(l + 1) * 64]
                tt(mraw, mtmp, colp[:, l * NCOL:l * NCOL + 64], ALU.add, ['mtmp', 'const'], [f'modc{l}'])
                gm = colp[:, l * NCOL + 64:l * NCOL + 80]
                gf = colp[:, l * NCOL + 80:l * NCOL + 96]
                stt(mraw[:, 16:32], mraw[:, 16:32], 1.0, gm, ALU.add, ALU.mult, [f'modc{l}', 'const'], [f'modc{l}'])
                stt(mraw[:, 48:64], mraw[:, 48:64], 1.0, gf, ALU.add, ALU.mult, [f'modc{l}', 'const'], [f'modc{l}'])
                for g_i, gi in enumerate([2, 5]):
                    for jb in range(4):
                        w = wb[wi % 2]
                        wk = f'adaw{wi % 2}'
                        wi += 1
                        S.dma('sp', w, awv[:, :, gi * D + jb * 512: gi * D + (jb + 1) * 512], (), [wk])
                        for kc in range(16):
                            mm(PS[1], condrep[:, kc, :], w[:, kc, :], kc == 0, kc == 15, [wk, f'crep{kc}'], [pk[1]])
                        c0 = (l * 2 + g_i) * D + jb * 512
                        S.dma('sp', brow, dr['adabr'][:, c0:c0 + 512], (), ['brow'])
                        o = orow[(jb) % 2]
                        ok_ = f'orow{jb % 2}'
                        tt(o, PS[1], brow, ALU.add, [pk[1], 'brow'], [ok_])
                        S.dma('sp', gate_d[:, c0:c0 + 512], o, [ok_], [f'gated{c0}'])
            S.barrier()
            A.pop()

        def mod_cols(l, which):
            base = l * 64 + (0 if which == 'm' else 32)
            return modc[:, base + 16:base + 32], modc[:, base:base + 16]

        def norm_T(src_d, dstT, dst_key, t0, t1, ngroups, acol, bcol, xt_bufs, junk, stat, hook=None):
            gw = D // ngroups
            for t in range(t0, t1):
                xt = xt_bufs[t % 2]
                xk = f'xt{t % 2}'
                S.dma('sp', xt, src_d[t * 128:(t + 1) * 128, :], (), [xk])
                ss = stat[:, 0:ngroups]
                rs = stat[:, 4:4 + ngroups]
                for g in range(ngroups):
                    act(junk[:, 0:gw], xt[:, g * gw:(g + 1) * gw], AF.Square, [xk], ['nstat'], accum_out=ss[:, g:g + 1])
                rstd_from_ss(rs, ss, gw, ['nstat'])
                for g in range(ngroups):
                    ts(xt[:, g * gw:(g + 1) * gw], xt[:, g * gw:(g + 1) * gw], rs[:, g:g + 1], None, ALU.mult, None, [xk, 'nstat'], [xk])
                for q4 in range(4):
                    p = PS[6 + (q4 % 2)]
                    pkk = pk[6 + (q4 % 2)]
                    for j in range(4):
                        dc = q4 * 4 + j
                        tr(p[:, j * 128:(j + 1) * 128], xt[:, dc * 128:(dc + 1) * 128], [xk], [pkk])
                    for j in range(4):
                        dc = q4 * 4 + j
                        o = dstT[:, dc, (t - t0) * 128:(t - t0 + 1) * 128]
                        if q4 % 2 == 1:
                            if bcol is None:
                                ts(o, p[:, j * 128:(j + 1) * 128], acol[:, dc:dc + 1], None, ALU.mult, None,
                                   [pkk, 'const', 'modc0', 'modc1'], [f'{dst_key}{t}'])
                            else:
                                ts(o, p[:, j * 128:(j + 1) * 128], acol[:, dc:dc + 1], bcol[:, dc:dc + 1], ALU.mult, ALU.add,
                                   [pkk, 'const', 'modc0', 'modc1'], [f'{dst_key}{t}'])
                        elif bcol is None:
                            act(o, p[:, j * 128:(j + 1) * 128], AF.Identity, [pkk, 'const', 'modc0', 'modc1'], [f'{dst_key}{t}'],
                                scale=acol[:, dc:dc + 1])
                        else:
                            act(o, p[:, j * 128:(j + 1) * 128], AF.Identity, [pkk, 'const', 'modc0', 'modc1'], [f'{dst_key}{t}'],
                                scale=acol[:, dc:dc + 1], bias=bcol[:, dc:dc + 1])
                        if hook is not None:
                            hook(t, dc, p[:, j * 128:(j + 1) * 128], pkk)

        wctr = [0]

        def load_w(bufs, key, src_ap, kcn, ncols):
            i = wctr[0] % len(bufs)
            wctr[0] += 1
            w = bufs[i][:, 0:kcn, 0:ncols]
            S.dma('pool', w, src_ap.rearrange("(kc p) n -> p kc n", p=128), (), [f'{key}{i}'])
            return w, f'{key}{i}'

        def phase_mixer(l, src_d):
            A.push()
            win = dr['w_in'][l]
            hT = A.bf16(16 * SEQ).rearrange("p (k t) -> p k t", t=SEQ)
            cmask = A.f32(512)
            smask = A.f32(512)
            umat = A.f32(128)
            ones = A.f32(128)
            invf = A.f32(96)
            rowp = A.f32(NROW)
            posi = A.f32(16).bitcast(I32)
            posf = A.f32(16)
            cs_pe = A.f32(2 * 16 * 32).rearrange("p (c t d) -> p c t d", c=2, t=16)
            cs_full = A.f32(2 * 16 * 64).rearrange("p (c t d) -> p c t d", c=2, t=16)
            wbufs = [A.bf16(16 * 256).rearrange("p (k n) -> p k n", n=256) for _ in range(2)]
            xt_bufs = [A.f32(D) for _ in range(2)]
            junk = A.bf16(D)
            stat = A.f32(16)
            for nm, ap_ in [('cmask', cmask), ('smask', smask), ('umat', umat), ('ones', ones), ('invf', invf)]:
                S.dma('sp', ap_, dr[nm], (), ['mconst'])
            S.dma('sp', rowp, dr['rowp'][:, l * NROW:(l + 1) * NROW], (), ['mconst'])
            S.dma('sp', posi, dr['pos'], (), ['posi'])
            cp(posf, posi, ['posi'], ['posf'])
            tmp_ang = xt_bufs[0]
            for (tab, i0, hd) in [(cs_pe, 0, 32), (cs_full, 32, 64)]:
                ang = tmp_ang[:, 0:16 * hd].rearrange("p (t d) -> p t d", d=hd)
                for t in range(16):
                    ts(ang[:, t, :], invf[:, i0:i0 + hd], posf[:, t:t + 1], None, ALU.mult, None, ['mconst', 'posf'], ['xt0'])
                for ci, shift in [(1, 0.0), (0, 0.25)]:
                    a2 = xt_bufs[1][:, 0:16 * hd].rearrange("p (t d) -> p t d", d=hd)
                    a3 = xt_bufs[1][:, 1024:1024 + 16 * hd].rearrange("p (t d) -> p t d", d=hd)
                    a3i = xt_bufs[1][:, 1024:1024 + 16 * hd].bitcast(I32).rearrange("p (t d) -> p t d", d=hd)
                    ts(a2, ang, 1.0 / (2 * math.pi), shift, ALU.mult, ALU.add, ['xt0'], ['xt1'])
                    cp(a3i, a2, ['xt1'], ['xt1'])
                    cp(a3, a3i, ['xt1'], ['xt1'])
                    tt(a2, a2, a3, ALU.subtract, ['xt1', 'xt1'], ['xt1'])
                    ts(a3, a2, 0.5, None, ALU.is_gt, None, ['xt1'], ['xt1'])
                    tt(a2, a2, a3, ALU.subtract, ['xt1', 'xt1'], ['xt1'])
                    act(tab[:, ci], a2, AF.Sin, ['xt1'], ['rope'], scale=2 * math.pi)
            am, bm = mod_cols(l, 'm')
            norm_T(src_d, hT, 'hT', 0, 16, 1, am, bm, xt_bufs, junk, stat)
            hkeys = [f'hT{t}' for t in range(16)]
            if l == 0:
                dbg_dump(S, 'd_hT', hT.rearrange("p k t -> p (k t)"), [128, 16 * SEQ], BF16, hkeys)

            ycv = ycat_d.rearrange("(t p) c -> p t c", p=128)

            def proj_tm(col0, ncols, cb):
                if ncols < 256:
                    w, wk = load_w(wbufs, 'win', win[:, col0:col0 + 256], 16, 256)
                    w = w[:, :, 0:ncols]
                else:
                    w, wk = load_w(wbufs, 'win', win[:, col0:col0 + ncols], 16, ncols)
                def grp(t):
                    pi = 6 + (t % 2)
                    for kc in range(16):
                        mm(PS[pi][:, 0:ncols], hT[:, kc, t * 128:(t + 1) * 128], w[:, kc, :], kc == 0, kc == 15,
                           [wk, f'hT{t}'], [pk[pi]])
                grp(0)
                pending = None
                for t in range(16):
                    if t + 1 < 16:
                        grp(t + 1)
                    pi = 6 + (t % 2)
                    fin2 = cb(t, PS[pi][:, 0:ncols], pk[pi])
                    if pending is not None:
                        pending()
                    pending = fin2
                if pending is not None:
                    pending()

            def proj_fm(col0, ncols, cb):
                w, wk = load_w(wbufs, 'win', win[:, col0:col0 + ncols], 16, ncols)
                for j in range(ncols // 128):
                    for tb in range(4):
                        pi = 6 + (tb % 2)
                        for kc in range(16):
                            mm(PS[pi], w[:, kc, j * 128:(j + 1) * 128], hT[:, kc, tb * 512:(tb + 1) * 512], kc == 0, kc == 15,
                               [wk] + hkeys[tb * 4:tb * 4 + 4], [pk[pi]])
                        cb(j, tb, PS[pi], pk[pi])

            def rope_tm(dst, src, skeys, tab, t, hd, tmpa, tmpb):
                cos = tab[:, 0, t, :]
                sin = tab[:, 1, t, :]
                x1 = src[:, 0:hd]
                x2 = src[:, hd:2 * hd]
                tt(tmpa[:, 0:hd], x1, cos, ALU.mult, skeys + ['rope'], ['ropetmp'])
                tt(tmpb[:, 0:hd], x2, sin, ALU.mult, skeys + ['rope'], ['ropetmp2'])
                tt(tmpa[:, hd:2 * hd], x2, cos, ALU.mult, skeys + ['rope'], ['ropetmp'])
                tt(tmpb[:, hd:2 * hd], x1, sin, ALU.mult, skeys + ['rope'], ['ropetmp2'])
                tt(dst[:, 0:hd], tmpa[:, 0:hd], tmpb[:, 0:hd], ALU.subtract, ['ropetmp', 'ropetmp2'], skeys)
                tt(dst[:, hd:2 * hd], tmpa[:, hd:2 * hd], tmpb[:, hd:2 * hd], ALU.add, ['ropetmp', 'ropetmp2'], skeys)

            def attn_core(nd, qT, kT, qkeys, kkeys, vt, vkeys, dvx, score_fn, descending, finish_fn):
                pairs = []
                for qb in range(4):
                    kts = list(range(0, 4 * qb + 4))
                    if descending:
                        kts = kts[::-1]
                    for idx, kt in enumerate(kts):
                        pairs.append((qb, kt, idx == len(kts) - 1))
                accs = [PS[4 + qs][:, 0:dvx] for qs in range(4)]
                acck = [pk[4 + qs] for qs in range(4)]

                def geom(i):
                    qb, kt, _ = pairs[i]
                    j = kt - 4 * qb
                    c0 = 128 * max(j, 0)
                    return qb, kt, j, c0, 512 - c0

                def emit_s(i):
                    qb, kt, j, c0, N = geom(i)
                    si = i % 4
                    ps_s = PS[si][:, 0:N]
                    for di in range(nd):
                        qa, kp = qT[di]
                        ka, _ = kT[di]
                        mm(ps_s, ka[0:kp, kt * 128:(kt + 1) * 128], qa[0:kp, qb * 512 + c0:(qb + 1) * 512], di == 0, di == nd - 1,
                           qkeys + kkeys, [pk[si]])
                emit_s(0)
                if len(pairs) > 1:
                    emit_s(1)
                for i in range(len(pairs)):
                    if i + 2 < len(pairs):
                        emit_s(i + 2)
                    qb, kt, j, c0, N = geom(i)
                    si = i % 4
                    PT = PTb[si]
                    ptk = f'PT{si}'
                    score_fn(qb, kt, j, c0, N, PS[si][:, 0:N], pk[si], PT, ptk)
                    for qs in range(max(j, 0), 4):
                        first = (kt == 4 * qb + qs) if descending else (kt == 0)
                        last = (kt == 0) if descending else (kt == 4 * qb + qs)
                        mm(accs[qs], PT[:, qs * 128:(qs + 1) * 128], vt(kt), first, last, [ptk] + vkeys, [acck[qs]], sig=True)
                    if pairs[i][2]:
                        finish_fn(qb, accs, acck)

            PTb = [A.bf16(512) for _ in range(4)]
            yout = [A.f32(512).rearrange("p (a b) -> p a b", b=128) for _ in range(2)]
            youtc = [0]

            def store_y(qb, col0, fill):
                i = youtc[0] % 2
                youtc[0] += 1
                yo = yout[i]
                fill(yo, f'yout{i}')
                S.dma('sp', ycv[:, 4 * qb:4 * qb + 4, col0:col0 + 128], yo, [f'yout{i}'], [f'ycat{qb}_{col0}'])

            mixer_base = A.off

            def mixer_sb():
                A.off = mixer_base
                QT = A.bf16(4 * SEQ).rearrange("p (h t) -> p h t", t=SEQ)
                KT = A.bf16(4 * SEQ).rearrange("p (h t) -> p h t", t=SEQ)
                V = A.bf16(16 * 512).rearrange("p (t c) -> p t c", c=512)
                t_e = A.f32(512)
                t_sp = A.f32(512)
                t_L = A.f32(512)
                t_1 = A.f32(512)
                carry = A.f32(512)
                for (dst, c0, nm) in [(QT, 2880, 'sbq'), (KT, 3392, 'sbk')]:
                    for half in range(2):
                        def cb(j, tb, ps, pkey, dst=dst, half=half, nm=nm):
                            act(dst[:, half * 2 + j, tb * 512:(tb + 1) * 512], ps, AF.Identity, [pkey], [f'{nm}{half * 2 + j}'])
                        proj_fm(c0 + half * 256, 256, cb)
                for half in range(2):
                    def cbv(t, ps, pkey, half=half):
                        act(V[:, t, half * 256:(half + 1) * 256], ps, AF.Identity, [pkey], ['sbv'])
                    proj_tm(3904 + half * 256, 256, cbv)
                scale = 128 ** -0.5
                tz2 = [A.f32(512) for _ in range(2)]
                tL2 = [A.f32(512) for _ in range(2)]
                Lacc = carry
                for h in range(4):
                    pairs = []
                    for qb in range(4):
                        kts = list(range(0, 4 * qb + 4))[::-1]
                        for idx, kt in enumerate(kts):
                            pairs.append((qb, kt, idx == 0, idx == len(kts) - 1))
                    accs = [PS[4 + qs][:, 0:128] for qs in range(4)]
                    acck = [pk[4 + qs] for qs in range(4)]
                    QTh, KTh = QT[:, h, :], KT[:, h, :]

                    def geom(i):
                        qb, kt, fst, lst = pairs[i]
                        j = kt - 4 * qb
                        c0 = 128 * max(j, 0)
                        return qb, kt, j, c0, 512 - c0, fst, lst

                    def stA(i):
                        qb, kt, j, c0, N, fst, lst = geom(i)
                        mm(PS[i % 2][:, 0:N], KTh[:, kt * 128:(kt + 1) * 128], QTh[:, qb * 512 + c0:(qb + 1) * 512], True, True,
                           [f'sbq{h}', f'sbk{h}'], [pk[i % 2]])

                    def stB(i):
                        qb, kt, j, c0, N, fst, lst = geom(i)
                        b2 = i % 2
                        ps_s, psk = PS[b2][:, 0:N], pk[b2]
                        tz, tzk, tL, tLk = tz2[b2], f'tz{b2}', tL2[b2], f'tL{b2}'
                        psw, pswk = PS[2 + b2][:, 0:N], pk[2 + b2]
                        if fst:
                            mset(Lacc, 0.0, ['Lacc'])
                        act(t_e[:, 0:N], ps_s, AF.Exp, [psk], ['t_e'], scale=scale)
                        act(t_sp[:, 0:N], t_e[:, 0:N], AF.Ln, ['t_e'], ['t_sp'], bias=1.0)
                        if j >= 0:
                            stt(tL[:, 0:N], t_sp[:, 0:N], -1.0, smask[:, 0:N], ALU.mult, ALU.mult, ['t_sp', 'mconst'], [tLk])
                        else:
                            ts(tL[:, 0:N], t_sp[:, 0:N], -1.0, None, ALU.mult, None, ['t_sp'], [tLk])
                        stt(tz[:, 0:N], ps_s, scale, t_sp[:, 0:N], ALU.mult, ALU.subtract, [psk, 't_sp'], [tzk])
                        mm(psw, umat, tL[:, 0:N], True, fst, ['mconst', tLk], [pswk], sig=True)
                        if not fst:
                            mm(psw, ones, Lacc[:, c0:512], False, True, ['mconst', 'Lacc'], [pswk], sig=True)
                        if not lst:
                            tt(Lacc[:, c0:512], Lacc[:, c0:512], tL[:, 0:N], ALU.add, ['Lacc', tLk], ['Lacc'], eng='pool')

                    def stC(i):
                        qb, kt, j, c0, N, fst, lst = geom(i)
                        b2 = i % 2
                        tz, tzk = tz2[b2], f'tz{b2}'
                        psw, pswk = PS[2 + b2][:, 0:N], pk[2 + b2]
                        PT, ptk = PTb[b2], f'PT{b2}'
                        tt(tz[:, 0:N], tz[:, 0:N], psw, ALU.add, [tzk, pswk], [tzk])
                        if j >= 0:
                            act(t_1[:, 0:N], tz[:, 0:N], AF.Exp, [tzk], ['t_1'])
                            tt(PT[:, c0:512], t_1[:, 0:N], smask[:, 0:N], ALU.mult, ['t_1', 'mconst'], [ptk])
                        else:
                            act(PT[:, c0:512], tz[:, 0:N], AF.Exp, [tzk], [ptk])
                        for qs in range(max(j, 0), 4):
                            mm(accs[qs], PT[:, qs * 128:(qs + 1) * 128], V[:, kt, h * 128:(h + 1) * 128], kt == 4 * qb + qs, kt == 0,
                               [ptk, 'sbv'], [acck[qs]], sig=True)
                        if lst:
                            def fill(yo, key):
                                for qs in range(4):
                                    act(yo[:, qs, :], accs[qs], AF.Identity, [acck[qs]], [key])
                            store_y(qb, 1024 + h * 128, fill)
                    npairs = len(pairs)
                    stA(0)
                    stA(1)
                    stB(0)
                    for i in range(npairs):
                        if i + 2 < npairs:
                            stA(i + 2)
                        if i + 1 < npairs:
                            stB(i + 1)
                        stC(i)

            def mixer_ret():
                A.off = mixer_base
                rett1 = A.f32(1024).rearrange("p (m x) -> p m x", m=2)
                QT = A.bf16(4 * SEQ).rearrange("p (h t) -> p h t", t=SEQ)
                KT = A.bf16(4 * SEQ).rearrange("p (h t) -> p h t", t=SEQ)
                V = A.bf16(16 * 512).rearrange("p (t c) -> p t c", c=512)
                SG = A.bf16(16 * 512).rearrange("p (t c) -> p t c", c=512)
                rt2 = [A.f32(256) for _ in range(2)]
                ra = A.f32(128)
                rb = A.f32(128)
                gst = A.f32(16)
                yc = A.f32(512).rearrange("p (a b) -> p a b", b=128)
                sgf = A.f32(512).rearrange("p (a b) -> p a b", b=128)
                for (dst, c0, nm) in [(QT, 832, 'rq'), (KT, 1344, 'rk')]:
                    for half in range(2):
                        def cb(t, ps, pkey, dst=dst, half=half, nm=nm):
                            rtt = rt2[t % 2]
                            rk = f'rt{t % 2}'
                            for hh in range(2):
                                rope_tm(rtt[:, hh * 128:(hh + 1) * 128], ps[:, hh * 128:(hh + 1) * 128], [pkey, rk], cs_full, t, 64, ra, rb)
                            p = PS[2 + (t % 2)]
                            for hh in range(2):
                                tr(p[:, hh * 128:(hh + 1) * 128], rtt[:, hh * 128:(hh + 1) * 128], [rk], [pk[2 + (t % 2)]])

                            def st2():
                                for hh in range(2):
                                    h = half * 2 + hh
                                    act(dst[:, h, t * 128:(t + 1) * 128], p[:, hh * 128:(hh + 1) * 128], AF.Identity, [pk[2 + (t % 2)]], [f'{nm}{h}'])
                            return st2
                        proj_tm(c0 + half * 256, 256, cb)
                for half in range(2):
                    def cbv(t, ps, pkey, half=half):
                        act(V[:, t, half * 256:(half + 1) * 256], ps, AF.Identity, [pkey], ['rv'])
                    proj_tm(1856 + half * 256, 256, cbv)
                for half in range(2):
                    def cbg(t, ps, pkey, half=half):
                        act(SG[:, t, half * 256:(half + 1) * 256], ps, AF.Silu, [pkey], ['rg'])
                    proj_tm(2368 + half * 256, 256, cbg)
                scale = 128 ** -0.5
                for h in range(4):
                    S.dma('sp', rett1.rearrange("p m x -> p (m x)"), dr['rett'][:, h * 1024:(h + 1) * 1024], (), ['rett'])

                    def score(qb, kt, j, c0, N, ps_s, psk, PT, ptk, h=h):
                        if j >= 0:
                            stt(PT[:, c0:512], ps_s, scale, rett1[:, 1, 0:N], ALU.mult, ALU.mult, [psk, 'rett'], [ptk])
                        else:
                            dl = 512 * qb - 128 * kt
                            stt(PT[:, c0:512], ps_s, scale * (GAMMA[h] ** dl), rett1[:, 0, 0:N], ALU.mult, ALU.mult, [psk, 'rett'], [ptk])

                    def fin(qb, accs, acck, h=h):
                        def fill(yo, key):
                            for qs in range(4):
                                act(yc[:, qs, :], accs[qs], AF.Identity, [acck[qs]], ['yc'], accum_out=gst[:, qs:qs + 1])
                            ts(gst[:, 4:8], gst[:, 0:4], 1.0 / 128, None, ALU.mult, None, ['yc'], ['yc'])
                            for qs in range(4):
                                ts(yc[:, qs, :], yc[:, qs, :], gst[:, 4 + qs:5 + qs], None, ALU.subtract, None, ['yc'], ['yc'])
                            for qs in range(4):
                                act(sgf[:, qs, :], yc[:, qs, :], AF.Square, ['yc'], ['sgf'], accum_out=gst[:, 8 + qs:9 + qs])
                            rstd_from_ss(gst[:, 12:16], gst[:, 8:12], 128, ['sgf', 'yc'])
                            for qs in range(4):
                                t = 4 * qb + qs
                                stt(yo[:, qs, :], yc[:, qs, :], gst[:, 12 + qs:13 + qs], SG[:, t, h * 128:(h + 1) * 128], ALU.mult, ALU.mult,
                                    ['yc', 'sgf', 'rg'], [key])
                        store_y(qb, 512 + h * 128, fill)
                    attn_core(1, [(QT[:, h, :], 128)], [(KT[:, h, :], 128)], [f'rq{h}'], [f'rk{h}'],
                              lambda kt, h=h: V[:, kt, h * 128:(h + 1) * 128], ['rv'], 128, score, False, fin)

            def mixer_moba():
                A.off = mixer_base
                mobac = A.f32(384).rearrange("p (m t n) -> p m t n", m=3, t=16)
                sel8 = A.f32(1024).rearrange("p (e m) -> p e m", m=128)
                QT = A.bf16(2 * SEQ).rearrange("p (h t) -> p h t", t=SEQ)
                KT = A.bf16(2 * SEQ).rearrange("p (h t) -> p h t", t=SEQ)
                V = A.bf16(16 * 2 * 130).rearrange("p (t h c) -> p t h c", t=16, h=2)
                selB = A.bf16(8 * SEQ).rearrange("p (n t) -> p n t", t=SEQ)
                selall = A.f32(128).rearrange("p (t n) -> p t n", n=8)
                selT = A.f32(SEQ)
                km = A.f32(8)
                kmb = A.bf16(8)
                gm = A.f32(8)
                top8 = A.f32(8)
                rt2 = [A.f32(256) for _ in range(2)]
                ra = A.f32(128)
                rb = A.f32(128)
                nst = A.f32(8)
                nstc = A.f32(8)
                t_e = A.f32(512)
                S.dma('sp', mobac.rearrange("p m t n -> p (m t n)"), dr['mobac'], (), ['mobac'])
                S.dma('sp', sel8[0:8].rearrange("p e m -> p (e m)"), dr['sel8'], (), ['sel8'])
                scale = 128 ** -0.5
                for half in range(2):
                    for hh in range(2):
                        mset(V[:, :, hh, 128:129], 1.0, ['mbv'])
                    for (dst, c0, nm, goff) in [(QT, 4416, 'mq', 384), (KT, 4928, 'mk', 512)]:
                        grow = rowp[:, goff:goff + 128]

                        def cb(t, ps, pkey, dst=dst, nm=nm, grow=grow):
                            rtt = rt2[t % 2]
                            rk = f'rt{t % 2}'
                            nk_ = f'nstc{t % 2}'
                            nc4 = nstc[:, (t % 2) * 4:(t % 2) * 4 + 4]
                            for hh in range(2):
                                sl = slice(hh * 128, (hh + 1) * 128)
                                act(junk[:, 0:128], ps[:, sl], AF.Square, [pkey], [nk_], accum_out=nc4[:, hh:hh + 1])
                            rstd_from_ss(nc4[:, 2:4], nc4[:, 0:2], 128, [nk_])
                            for hh in range(2):
                                sl = slice(hh * 128, (hh + 1) * 128)
                                stt(rtt[:, sl], ps[:, sl], nc4[:, 2 + hh:3 + hh], grow, ALU.mult, ALU.mult, [pkey, nk_, 'mconst'], [rk])
                                rope_tm(rtt[:, sl], rtt[:, sl], [rk], cs_full, t, 64, ra, rb)
                            p = PS[2 + (t % 2)]
                            for hh in range(2):
                                tr(p[:, hh * 128:(hh + 1) * 128], rtt[:, hh * 128:(hh + 1) * 128], [rk], [pk[2 + (t % 2)]])

                            def st2():
                                for hh in range(2):
                                    act(dst[:, hh, t * 128:(t + 1) * 128], p[:, hh * 128:(hh + 1) * 128], AF.Identity, [pk[2 + (t % 2)]], [f'{nm}{hh}'])
                            return st2
                        proj_tm(c0 + half * 256, 256, cb)

                    def cbv(t, ps, pkey):
                        for hh in range(2):
                            act(V[:, t, hh, 0:128], ps[:, hh * 128:(hh + 1) * 128], AF.Identity, [pkey], ['mbv'])
                    proj_tm(5440 + half * 256, 256, cbv)
                    for hh in range(2):
                        h = half * 2 + hh
                        S.op('dve', lambda e, hh=hh: e.tensor_reduce(out=km, in_=KT[:, hh, :].rearrange("p (n k) -> p n k", k=256),
                                                                      axis=AX.X, op=ALU.add), [f'mk{hh}'], ['km'])
                        ts(kmb, km, 1.0 / 256, None, ALU.mult, None, ['km'], ['kmb'])
                        mm(PS[2][:, 0:8], QT[:, hh, 0:128], kmb, True, True, [f'mq{hh}', 'kmb'], [pk[2]])
                        for t in range(16):
                            pg = PS[2 + (t % 2)]
                            if t + 1 < 16:
                                mm(PS[2 + ((t + 1) % 2)][:, 0:8], QT[:, hh, (t + 1) * 128:(t + 2) * 128], kmb, True, True,
                                   [f'mq{hh}', 'kmb'], [pk[2 + ((t + 1) % 2)]])
                            tt(gm, pg[:, 0:8], mobac[:, 0, t, :], ALU.mult, [pk[2 + (t % 2)], 'mobac'], ['gm'])
                            tt(gm, gm, mobac[:, 1, t, :], ALU.add, ['gm', 'mobac'], ['gm'])
                            S.op('dve', lambda e: e.max(out=top8, in_=gm), ['gm'], ['top8'])
                            ts(selall[:, t, :], gm, top8[:, 2:3], None, ALU.is_ge, None, ['gm', 'top8'], ['selall'])
                            tt(selall[:, t, :], selall[:, t, :], mobac[:, 0, t, :], ALU.mult, ['selall', 'mobac'], ['selall'])
                            tt(selall[:, t, :], selall[:, t, :], mobac[:, 2, t, :], ALU.max, ['selall', 'mobac'], ['selall'])
                            tr(pg[0:8, 128:256], selall[:, t, :], ['selall'], [pk[2 + (t % 2)]])
                            cp(selT[0:8, t * 128:(t + 1) * 128], pg[0:8, 128:256], [pk[2 + (t % 2)]], ['selT'])
                        for n in range(8):
                            for qb in range(n // 2, 4):
                                pi = 2 + ((n * 4 + qb) % 2)
                                mm(PS[pi], sel8[0:8, n, :], selT[0:8, qb * 512:(qb + 1) * 512], True, True, ['sel8', 'selT'], [pk[pi]])
                                act(selB[:, n, qb * 512:(qb + 1) * 512], PS[pi], AF.Identity, [pk[pi]], ['selB'])

                        def score(qb, kt, j, c0, N, ps_s, psk, PT, ptk):
                            n = kt // 2
                            need_sel = (qb >= 2) and (j < 2)
                            if not need_sel and j < 0:
                                act(PT[:, c0:512], ps_s, AF.Exp, [psk], [ptk], scale=scale)
                                return
                            act(t_e[:, 0:N], ps_s, AF.Exp, [psk], ['t_e'], scale=scale)
                            if not need_sel:
                                tt(PT[:, c0:512], t_e[:, 0:N], cmask[:, 0:N], ALU.mult, ['t_e', 'mconst'], [ptk])
                                return
                            if j >= 0:
                                tt(t_e[:, 0:N], t_e[:, 0:N], cmask[:, 0:N], ALU.mult, ['t_e', 'mconst'], ['t_e'])
                            tt(PT[:, c0:512], t_e[:, 0:N], selB[:, n, qb * 512 + c0:(qb + 1) * 512], ALU.mult, ['t_e', 'selB'], [ptk])

                        def fin(qb, accs, acck, h=h):
                            def fill(yo, key):
                                for qs in range(4):
                                    S.op('dve', lambda e, qs=qs: e.reciprocal(out=nst[:, 4 + qs:5 + qs], in_=accs[qs][:, 128:129]), [acck[qs]], ['nst2'])
                                    ts(yo[:, qs, :], accs[qs][:, 0:128], nst[:, 4 + qs:5 + qs], None, ALU.mult, None, [acck[qs], 'nst2'], [key])
                            store_y(qb, 1536 + h * 128, fill)
                        attn_core(1, [(QT[:, hh, :], 128)], [(KT[:, hh, :], 128)], [f'mq{hh}'], [f'mk{hh}', 'selB'],
                                  lambda kt, hh=hh: V[:, kt, hh, 0:129], ['mbv'], 129, score, False, fin)

            def mixer_mla():
                A.off = mixer_base
                cqnT = A.bf16(4 * SEQ).rearrange("p (k t) -> p k t", t=SEQ)
                ckvnT = A.bf16(2 * SEQ).rearrange("p (k t) -> p k t", t=SEQ)
                kr = A.f32(16 * 64).rearrange("p (t d) -> p t d", d=64)
                sskpe = A.f32(16)
                QT = A.bf16(2 * SEQ).rearrange("p (k t) -> p k t", t=SEQ)
                KT = A.bf16(2 * SEQ).rearrange("p (k t) -> p k t", t=SEQ)
                V = A.bf16(16 * 130).rearrange("p (t c) -> p t c", c=130)
                wuq = A.bf16(4 * 192).rearrange("p (k n) -> p k n", n=192)
                wukv = A.bf16(2 * 256).rearrange("p (k n) -> p k n", n=256)
                cq = A.f32(512)
                rt = A.f32(192)
                ra = A.f32(64)
                rb = A.f32(64)
                kn = A.f32(192)
                nst = A.f32(16)
                t_e = A.f32(512)
                gq = rowp[:, 0:192]
                gk = rowp[:, 192:384]
                qng = colp[:, l * NCOL + 96:l * NCOL + 100]
                kvng = colp[:, l * NCOL + 100:l * NCOL + 102]

                def cb_cq(half):
                    def cb(t, ps, pkey):
                        act(junk[:, 0:256], ps, AF.Square, [pkey], ['ssq'], accum_out=nst[:, half:half + 1])
                        cp(cq[:, half * 256:(half + 1) * 256], ps, [pkey, 'ssq'], ['cq'])
                        if half == 1:
                            tt(nst[:, 2:3], nst[:, 0:1], nst[:, 1:2], ALU.add, ['ssq'], ['ssq'])
                            rstd_from_ss(nst[:, 3:4], nst[:, 2:3], 512, ['ssq'])
                            act(cq, cq, AF.Identity, ['cq', 'ssq'], ['cq'], scale=nst[:, 3:4])
                            p = PS[2 + (t % 2)]
                            for kc in range(4):
                                tr(p[:, kc * 128:(kc + 1) * 128], cq[:, kc * 128:(kc + 1) * 128], ['cq'], [pk[2 + (t % 2)]])
                            for kc in range(4):
                                act(cqnT[:, kc, t * 128:(t + 1) * 128], p[:, kc * 128:(kc + 1) * 128], AF.Identity, [pk[2 + (t % 2)], 'const'],
                                    ['cqnT'], scale=qng[:, kc:kc + 1])
                    return cb
                w0, wk0 = load_w(wbufs, 'win', win[:, 0:256], 16, 256)
                w1, wk1 = load_w(wbufs, 'win', win[:, 256:512], 16, 256)
                def grp_cq(t):
                    for half, (w, wk) in enumerate([(w0, wk0), (w1, wk1)]):
                        pi = 4 + 2 * (t % 2) + half
                        for kc in range(16):
                            mm(PS[pi][:, 0:256], hT[:, kc, t * 128:(t + 1) * 128], w[:, kc, :], kc == 0, kc == 15, [wk, f'hT{t}'], [pk[pi]])
                grp_cq(0)
                for t in range(16):
                    if t + 1 < 16:
                        grp_cq(t + 1)
                    for half in range(2):
                        pi = 4 + 2 * (t % 2) + half
                        cb_cq(half)(t, PS[pi][:, 0:256], pk[pi])

                import os
                if int(os.environ.get('MLA_STAGE', '9')) <= 0:
                    return

                def cb_ckv(t, ps, pkey):
                    act(junk[:, 0:256], ps, AF.Square, [pkey], ['sskv'], accum_out=nst[:, 4:5])
                    rstd_from_ss(nst[:, 5:6], nst[:, 4:5], 256, ['sskv'])
                    act(cq[:, 0:256], ps, AF.Identity, [pkey, 'sskv'], ['cq'], scale=nst[:, 5:6])
                    p = PS[2 + (t % 2)]
                    for kc in range(2):
                        tr(p[:, kc * 128:(kc + 1) * 128], cq[:, kc * 128:(kc + 1) * 128], ['cq'], [pk[2 + (t % 2)]])
                    for kc in range(2):
                        act(ckvnT[:, kc, t * 128:(t + 1) * 128], p[:, kc * 128:(kc + 1) * 128], AF.Identity, [pk[2 + (t % 2)], 'const'],
                            ['ckvnT'], scale=kvng[:, kc:kc + 1])
                proj_tm(512, 256, cb_ckv)

                def cb_kpe(t, ps, pkey):
                    act(junk[:, 0:64], ps, AF.Square, [pkey], ['sskpe'], accum_out=sskpe[:, t:t + 1])
                    tt(rt[:, 0:64], ps, gk[:, 128:192], ALU.mult, [pkey, 'mconst'], ['rt'])
                    rope_tm(kr[:, t, :], rt[:, 0:64], ['rt', 'kr'], cs_pe, t, 32, ra, rb)
                proj_tm(768, 64, cb_kpe)

                scale = 192 ** -0.5
                kbias = A.f32(16)
                ts(kbias, sskpe, 1.0 / 192, EPS, ALU.mult, ALU.add, ['sskpe'], ['kbias'])
                import os
                MST = int(os.environ.get('MLA_STAGE', '9'))
                if MST <= 1:
                    return
                wuq_d = dr['mla_w_uq'][l]
                wukv_d = dr['mla_w_ukv'][l]
                for h in range(4):
                    S.dma('pool', wuq, wuq_d[:, h * 192:(h + 1) * 192].rearrange("(kc p) n -> p kc n", p=128), (), ['wuq'])
                    S.dma('pool', wukv, wukv_d[:, h * 256:(h + 1) * 256].rearrange("(kc p) n -> p kc n", p=128), (), ['wukv'])
                    if h == 0:
                        mset(V[:, :, 128:129], 1.0, ['mlav'])
                    def up(t):
                        bq = 4 + 2 * (t % 2)
                        for kc in range(4):
                            mm(PS[bq][:, 0:192], cqnT[:, kc, t * 128:(t + 1) * 128], wuq[:, kc, :], kc == 0, kc == 3, ['cqnT', 'wuq'], [pk[bq]])
                        for kc in range(2):
                            mm(PS[bq + 1][:, 0:256], ckvnT[:, kc, t * 128:(t + 1) * 128], wukv[:, kc, :], kc == 0, kc == 1, ['ckvnT', 'wukv'], [pk[bq + 1]])
                    up(0)
                    for t in range(16):
                        if t + 1 < 16:
                            up(t + 1)
                        bq = 4 + 2 * (t % 2)
                        pq = PS[bq]
                        pkq = pk[bq]
                        pkv = PS[bq + 1]
                        pkvk = pk[bq + 1]
                        act(junk[:, 0:192], pq[:, 0:192], AF.Square, [pkq], ['nq'], accum_out=nst[:, 6:7])
                        rstd_from_ss(nst[:, 7:8], nst[:, 6:7], 192, ['nq'])
                        act(junk[:, 0:128], pkv[:, 0:128], AF.Square, [pkvk], ['nk'], accum_out=nst[:, 8:9])
                        act(V[:, t, 0:128], pkv[:, 128:256], AF.Identity, [pkvk], ['mlav'])
                        act(nst[:, 9:10], nst[:, 8:9], AF.Ln, ['nk', 'kbias'], ['nk'], scale=1.0 / 192, bias=kbias[:, t:t + 1])
                        act(nst[:, 9:10], nst[:, 9:10], AF.Exp, ['nk'], ['nk'], scale=-0.5)
                        stt(rt, pq[:, 0:192], nst[:, 7:8], gq, ALU.mult, ALU.mult, [pkq, 'nq', 'mconst'], ['rt'])
                        rope_tm(rt[:, 128:192], rt[:, 128:192], ['rt'], cs_pe, t, 32, ra, rb)
                        p = PS[2 + (t % 2)]
                        tr(p[:, 0:128], rt[:, 0:128], ['rt'], [pk[2 + (t % 2)]])
                        tr(p[0:64, 128:256], rt[:, 128:192], ['rt'], [pk[2 + (t % 2)]])
                        stt(kn[:, 0:128], pkv[:, 0:128], nst[:, 9:10], gk[:, 0:128], ALU.mult, ALU.mult, [pkvk, 'nk', 'mconst'], ['kn'])
                        ts(kn[:, 128:192], kr[:, t, :], nst[:, 9:10], None, ALU.mult, None, ['kr', 'nk'], ['kn'])
                        p2 = PS[2 + ((t + 1) % 2)]
                        tr(p2[:, 256:384], kn[:, 0:128], ['kn'], [pk[2 + ((t + 1) % 2)]])
                        tr(p2[0:64, 384:512], kn[:, 128:192], ['kn'], [pk[2 + ((t + 1) % 2)]])
                        act(QT[:, 0, t * 128:(t + 1) * 128], p[:, 0:128], AF.Identity, [pk[2 + (t % 2)]], ['mlaq'])
                        act(QT[0:64, 1, t * 128:(t + 1) * 128], p[0:64, 128:256], AF.Identity, [pk[2 + (t % 2)]], ['mlaq'])
                        act(KT[:, 0, t * 128:(t + 1) * 128], p2[:, 256:384], AF.Identity, [pk[2 + ((t + 1) % 2)]], ['mlak'])
                        act(KT[0:64, 1, t * 128:(t + 1) * 128], p2[0:64, 384:512], AF.Identity, [pk[2 + ((t + 1) % 2)]], ['mlak'])

                    def score(qb, kt, j, c0, N, ps_s, psk, PT, ptk):
                        if j >= 0:
                            act(t_e[:, 0:N], ps_s, AF.Exp, [psk], ['t_e'], scale=scale)
                            tt(PT[:, c0:512], t_e[:, 0:N], cmask[:, 0:N], ALU.mult, ['t_e', 'mconst'], [ptk])
                        else:
                            act(PT[:, c0:512], ps_s, AF.Exp, [psk], [ptk], scale=scale)

                    def fin(qb, accs, acck, h=h):
                        def fill(yo, key):
                            for qs in range(4):
                                S.op('dve', lambda e, qs=qs: e.reciprocal(out=nst[:, 10 + qs:11 + qs], in_=accs[qs][:, 128:129]), [acck[qs]], ['nst2'])
                                ts(yo[:, qs, :], accs[qs][:, 0:128], nst[:, 10 + qs:11 + qs], None, ALU.mult, None, [acck[qs], 'nst2'], [key])
                        store_y(qb, h * 128, fill)
                    if MST <= 2:
                        continue
                    attn_core(2, [(QT[:, 0, :], 128), (QT[:, 1, :], 64)], [(KT[:, 0, :], 128), (KT[:, 1, :], 64)], ['mlaq'], ['mlak'],
                              lambda kt: V[:, kt, 0:129], ['mlav'], 129, score, False, fin)

            which = MIXERS
            if 'mla' in which:
                mixer_mla()
                S.barrier()
            if 'ret' in which:
                mixer_ret()
                S.barrier()
            if 'sb' in which:
                mixer_sb()
                S.barrier()
            if 'moba' in which:
                mixer_moba()
                S.barrier()

            A.off = mixer_base
            gateB = A.f32(D)
            xs = [A.f32(512) for _ in range(3)]
            ys = [A.f32(512) for _ in range(3)]
            wob = [A.bf16(16 * 512).rearrange("p (k n) -> p k n", n=512) for _ in range(2)]
            S.dma('sp', gateB, gate_d[:, (l * 2) * D:(l * 2 + 1) * D], (), ['gateB'])
            gng = colp[:, l * NCOL + 102:l * NCOL + 118]
            norm_T(ycat_d, hT, 'hT', 0, 16, 4, gng, None, xt_bufs, junk, stat)
            wout = dr['w_out'][l]
            ctr = 0
            for cb_ in range(4):
                w, wk = load_w(wob, 'wob', wout[:, cb_ * 512:(cb_ + 1) * 512], 16, 512)

                def grp_o(t, w=w, wk=wk):
                    pi = 6 + (t % 2)
                    for kc in range(16):
                        mm(PS[pi], hT[:, kc, t * 128:(t + 1) * 128], w[:, kc, :], kc == 0, kc == 15, [wk, f'hT{t}'], [pk[pi]])
                grp_o(0)
                for t in range(16):
                    if t + 1 < 16:
                        grp_o(t + 1)
                    pi = 6 + (t % 2)
                    i = ctr % 3
                    ctr += 1
                    S.dma('sp', xs[i], src_d[t * 128:(t + 1) * 128, cb_ * 512:(cb_ + 1) * 512], (), [f'xs{i}'])
                    tt(ys[i], PS[pi], gateB[:, cb_ * 512:(cb_ + 1) * 512], ALU.mult, [pk[pi], 'gateB'], [f'ys{i}'])
                    tt(ys[i], ys[i], xs[i], ALU.add, [f'ys{i}', f'xs{i}'], [f'ys{i}'])
                    S.dma('sp', x1_d[t * 128:(t + 1) * 128, cb_ * 512:(cb_ + 1) * 512], ys[i], [f'ys{i}'], [f'x1d{t}_{cb_}'])
            S.barrier()
            A.pop()

        def phase_ffn(l, src_d, dst_d):
            A.push()
            moe = (l == 1)
            nexp = 8 if moe else 1
            h2T = A.bf16(16 * 1024).rearrange("p (k t) -> p k t", t=1024)
            gT = A.bf16(11 * 1024).rearrange("p (f t) -> p f t", t=1024)
            yacc = A.f32(8 * D).rearrange("p (t d) -> p t d", d=D)
            w1b = [A.bf16(16 * 128).rearrange("p (k n) -> p k n", n=128) for _ in range(2)]
            w3b = [A.bf16(16 * 128).rearrange("p (k n) -> p k n", n=128) for _ in range(2)]
            w2b = [A.bf16(11 * 512).rearrange("p (f n) -> p f n", n=512) for _ in range(2)]
            xt_bufs = [A.f32(D) for _ in range(2)]
            junk = A.bf16(D)
            stat = A.f32(16)
            gateB = A.f32(D)
            su = A.f32(512)
            S.dma('sp', gateB, gate_d[:, (l * 2 + 1) * D:(l * 2 + 2) * D], (), ['gateB'])
            if moe:
                sel8 = A.f32(1024).rearrange("p (e m) -> p e m", m=128)
                rw = A.f32(16 * 8).rearrange("p (k e) -> p k e", e=8)
                hf = A.f32(128)
                lg = A.f32(8)
                top8 = A.f32(8)
                gsel = A.f32(8)
                G = A.f32(8 * 8).rearrange("p (t e) -> p t e", e=8)
                GT = A.f32(1024)
                egate = A.f32(1024)
                mst = A.f32(8)
                S.dma('sp', sel8[0:8].rearrange("p e m -> p (e m)"), dr['sel8'], (), ['sel8'])
                S.dma('sp', rw, dr['router_w'][0].rearrange("(kc p) e -> p kc e", p=128), (), ['rw'])
            af, bf = mod_cols(l, 'f')
            for th in range(2):
                hook = None
                if moe:
                    def hook(t, dc, ps, pkey, th=th):
                        tl = t - th * 8
                        act(hf, ps, AF.Identity, [pkey, 'modc1'], ['hf'], scale=af[:, dc:dc + 1], bias=bf[:, dc:dc + 1])
                        mm(PS[2][:, tl * 8:tl * 8 + 8], hf, rw[:, dc, :], dc == 0, dc == 15, ['hf', 'rw'], [pk[2]], sig=True)
                norm_T(src_d, h2T, f'h2T{th}_', th * 8, th * 8 + 8, 1, af, bf, xt_bufs, junk, stat, hook)
                hk = [f'h2T{th}_{t}' for t in range(th * 8, th * 8 + 8)]
                if moe:
                    for tl in range(8):
                        cp(lg, PS[2][:, tl * 8:tl * 8 + 8], [pk[2]], ['lg'])
                        S.op('dve', lambda e: e.max(out=top8, in_=lg), ['lg'], ['top8'])
                        tt(mst[:, 0:1], top8[:, 1:2], top8[:, 0:1], ALU.subtract, ['top8'], ['mst'])
                        act(mst[:, 1:2], mst[:, 0:1], AF.Exp, ['mst'], ['mst'])
                        ts(mst[:, 1:2], mst[:, 1:2], 1.0, None, ALU.add, None, ['mst'], ['mst'])
                        S.op('dve', lambda e: e.reciprocal(out=mst[:, 2:3], in_=mst[:, 1:2]), ['mst'], ['mst'])
                        ts(gsel, lg, top8[:, 1:2], None, ALU.is_ge, None, ['lg', 'top8'], ['gsel'])
                        ts(mst[:, 3:4], top8[:, 0:1], -1.0, None, ALU.mult, None, ['top8', 'mst'], ['mst'])
                        act(lg, lg, AF.Exp, ['lg', 'mst'], ['lg'], bias=mst[:, 3:4])
                        stt(G[:, tl, :], lg, mst[:, 2:3], gsel, ALU.mult, ALU.mult, ['lg', 'mst', 'gsel'], ['G'])
                        if tl < 4:
                            tr(PS[3][0:8, tl * 128:(tl + 1) * 128], G[:, tl, :], ['G'], [pk[3]])
                        else:
                            tr(PS[1][0:8, (tl - 4) * 128:(tl - 3) * 128], G[:, tl, :], ['G'], [pk[1]])
                    cp(GT[0:8, 0:512], PS[3][0:8, :], [pk[3]], ['GT'])
                    cp(GT[0:8, 512:1024], PS[1][0:8, :], [pk[1]], ['GT'])
                for e in range(nexp):
                    if moe:
                        w1d, w3d, w2d = dr['moe_w1'][0, e], dr['moe_w3'][0, e], dr['moe_w2'][0, e]
                        for tb in range(2):
                            mm(PS[3], sel8[0:8, e, :], GT[0:8, tb * 512:(tb + 1) * 512], True, True, ['sel8', 'GT'], [pk[3]])
                            act(egate[:, tb * 512:(tb + 1) * 512], PS[3], AF.Identity, [pk[3]], ['egate'])
                    else:
                        w1d, w3d, w2d = dr['ffn_w1'][0], dr['ffn_w3'][0], dr['ffn_w2'][0]
                    for fq in range(4):
                        for fl in range(11):
                            fc = fq * 11 + fl
                            w1, k1 = load_w(w1b, 'w1', w1d[:, fc * 128:(fc + 1) * 128], 16, 128)
                            wctr[0] -= 1
                            w3, k3 = load_w(w3b, 'w3', w3d[:, fc * 128:(fc + 1) * 128], 16, 128)
                            for tb in range(2):
                                pu = PS[0 + tb]
                                pv = PS[4 + tb]
                                for kc in range(16):
                                    mm(pu, w1[:, kc, :], h2T[:, kc, tb * 512:(tb + 1) * 512], kc == 0, kc == 15, [k1] + hk[tb * 4:tb * 4 + 4], [pk[tb]])
                                for kc in range(16):
                                    mm(pv, w3[:, kc, :], h2T[:, kc, tb * 512:(tb + 1) * 512], kc == 0, kc == 15, [k3] + hk[tb * 4:tb * 4 + 4], [pk[4 + tb]])
                                act(su, pu, AF.Silu, [pk[tb]], ['su'])
                                if moe:
                                    tt(su, su, egate[:, tb * 512:(tb + 1) * 512], ALU.mult, ['su', 'egate'], ['su'])
                                tt(gT[:, fl, tb * 512:(tb + 1) * 512], su, pv, ALU.mult, ['su', pk[4 + tb]], [f'gT{tb}'])
                        for db in range(4):
                            i2 = (fq * 4 + db) % 2
                            w2 = w2b[i2]
                            k2 = f'w2_{i2}'
                            S.dma('pool', w2, w2d[fq * 1408:(fq + 1) * 1408, db * 512:(db + 1) * 512].rearrange("(f p) n -> p f n", p=128), (), [k2])
                            for tl in range(8):
                                pi = 6 + (tl % 2)
                                for fl in range(11):
                                    mm(PS[pi], gT[:, fl, tl * 128:(tl + 1) * 128], w2[:, fl, :], fl == 0, fl == 10, [k2, f'gT{tl // 4}'], [pk[pi]])
                                ya = yacc[:, tl, db * 512:(db + 1) * 512]
                                if e == 0 and fq == 0:
                                    cp(ya, PS[pi], [pk[pi]], [f'yacc{tl}'])
                                else:
                                    tt(ya, ya, PS[pi], ALU.add, [pk[pi], f'yacc{tl}'], [f'yacc{tl}'])
                for tl in range(8):
                    t = th * 8 + tl
                    xt = xt_bufs[t % 2]
                    xk = f'xt{t % 2}'
                    S.dma('sp', xt, src_d[t * 128:(t + 1) * 128, :], (), [xk])
                    tt(yacc[:, tl, :], yacc[:, tl, :], gateB, ALU.mult, [f'yacc{tl}', 'gateB'], [f'yacc{tl}'])
                    tt(xt, xt, yacc[:, tl, :], ALU.add, [xk, f'yacc{tl}'], [xk])
                    S.dma('sp', dst_d[t * 128:(t + 1) * 128, :], xt, [xk], [f'dst{t}'])
            S.barrier()
            A.pop()

        phase_ada()
        stages = [('mix', 0, dr['x']), ('ffn', 0, x1_d, x2_d), ('mix', 1, x2_d), ('ffn', 1, x1_d, out_d)]
        n = len(stages) if stop_after is None else stop_after
        for st in stages[:n]:
            if st[0] == 'mix':
                phase_mixer(st[1], st[2])
            else:
                phase_ffn(st[1], st[2], st[3])
        S.barrier()
        S.emit_all()
    return nc


MIXERS = ('mla', 'ret', 'sb', 'moba')


def host_inputs(inputs):
    f = lambda a: np.ascontiguousarray(np.asarray(a, dtype=np.float32))
    consts = host_consts()
    colp = np.zeros((128, 2 * NCOL), np.float32)
    rowp = np.zeros((128, 2 * NROW), np.float32)
    adabr = np.zeros((128, 4 * D), np.float32)

    def col(v):
        return np.asarray(v, np.float32).reshape(-1, 128).T

    for l in range(2):
        ab = np.asarray(inputs['ada_b'][l], np.float32)
        o = l * NCOL
        for i, gi in enumerate([0, 1, 3, 4]):
            colp[:, o + i * 16:o + (i + 1) * 16] = col(ab[gi * D:(gi + 1) * D])
        colp[:, o + 64:o + 80] = col(inputs['norm_mix_g'][l])
        colp[:, o + 80:o + 96] = col(inputs['norm_ffn_g'][l])
        colp[:, o + 96:o + 100] = col(inputs['mla_q_norm_g'][l])
        colp[:, o + 100:o + 102] = col(inputs['mla_kv_norm_g'][l])
        colp[:, o + 102:o + 118] = col(np.asarray(inputs['group_norm_g'][l]).reshape(-1))
        r = l * NROW
        rowp[:, r:r + 192] = np.asarray(inputs['mla_q_head_g'][l], np.float32)[None, :]
        rowp[:, r + 192:r + 384] = np.asarray(inputs['mla_k_head_g'][l], np.float32)[None, :]
        rowp[:, r + 384:r + 512] = np.asarray(inputs['moba_q_head_g'][l], np.float32)[None, :]
        rowp[:, r + 512:r + 640] = np.asarray(inputs['moba_k_head_g'][l], np.float32)[None, :]
        adabr[:, (l * 2) * D:(l * 2 + 1) * D] = ab[2 * D:3 * D][None, :]
        adabr[:, (l * 2 + 1) * D:(l * 2 + 2) * D] = ab[5 * D:6 * D][None, :]
    shared = {'colp': colp, 'rowp': rowp, 'adabr': adabr}
    shared.update(consts)
    for k, _ in WEIGHTS:
        shared[k] = f(inputs[k])
    pos = np.ascontiguousarray(np.asarray(inputs['positions'], np.int32).reshape(16, 128).T)
    x = np.asarray(inputs['x'], np.float32)
    c = np.asarray(inputs['c'], np.float32)
    maps = []
    for b in range(8):
        m = dict(shared)
        m['x'] = np.ascontiguousarray(x[b])
        m['c_col'] = np.ascontiguousarray(c[b].reshape(16, 128).T)
        m['pos'] = pos
        maps.append(m)
    return maps


_NC_CACHE = {}


def kernel(**inputs):
    maps = host_inputs(inputs)
    if 'nc' not in _NC_CACHE:
        _NC_CACHE['nc'] = build()
    nc = _NC_CACHE['nc']
    res = run_bass_kernel_spmd(nc, maps, core_ids=list(range(8)))
    return np.stack([np.asarray(r['out'], dtype=np.float32) for r in res.results], axis=0)
```
